# Optimizing a Trainium2 kernel written in Bass

```python
import math
import jax, jax.numpy as jnp
from jax import lax
import numpy as np

D_MODEL = 1024
BATCH = 16
SEQ = 2048
DEPTH = 4

HEAD_DIM = 64
N_HEADS = D_MODEL // HEAD_DIM
D_FF = 2816
NORM_EPS = 1e-6
N_MIXERS = 3

REL_BUCKETS = 32
REL_MAX_DIST = 1024

QBLK = 128

A_KV_HEADS = 2
A_WINDOW = 128

B_KV_GROUPS = 4
CMP_LEN = 32
CMP_STRIDE = 16
CMP_HIDDEN = 256
SLC_BLK = 64
SLC_TOPK = 16
B_WINDOW = 512
B_QCHUNK = 128

MOBA_BLK = 256
MOBA_TOPK = 3
MOBA_QCHUNK = 64

NEG = -1e30
TINY = 1e-30

A_IN = N_HEADS * HEAD_DIM + 2 * A_KV_HEADS * HEAD_DIM
B_IN = N_HEADS * HEAD_DIM + 6 * B_KV_GROUPS * HEAD_DIM + 3 * N_HEADS
C_IN = 3 * N_HEADS * HEAD_DIM

N_A_LAYERS = (DEPTH + 2) // 3
N_B_LAYERS = (DEPTH + 1) // 3
N_C_LAYERS = DEPTH // 3

kernel_name = "hybrid_swa_nsa_moba_macaron"


def rms_norm(x, g):
    xf = x.astype(jnp.float32)
    y = xf * lax.rsqrt(jnp.mean(xf * xf, axis=-1, keepdims=True) + NORM_EPS)
    return (y * g.astype(jnp.float32)).astype(x.dtype)


def swiglu(h, w_in, w_out):
    gate, up = jnp.split(h @ w_in, 2, axis=-1)
    return (jax.nn.silu(gate) * up) @ w_out


def rel_bucket(dist):
    dist = jnp.maximum(dist, 0)
    exact = REL_BUCKETS // 2
    d = jnp.maximum(dist, 1).astype(jnp.float32)
    log_b = exact + (jnp.log(d / exact) / math.log(REL_MAX_DIST / exact)
                     * (REL_BUCKETS - exact)).astype(jnp.int32)
    return jnp.where(dist < exact, dist, jnp.minimum(log_b, REL_BUCKETS - 1))


def masked_softmax(s, ok, sink=None):
    s = jnp.where(ok, s, NEG)
    m = jnp.max(s, axis=-1, keepdims=True)
    if sink is not None:
        m = jnp.maximum(m, sink)
    e = jnp.where(ok, jnp.exp(s - m), 0.0)
    denom = jnp.sum(e, axis=-1, keepdims=True)
    if sink is not None:
        denom = denom + jnp.exp(sink - m)
    return e / jnp.maximum(denom, TINY)


def banded_attention(q, k, v, rel_table, window, sinks=None):
    B, S, H, dh = q.shape
    G = k.shape[2]
    R = H // G
    n_prev = -(-(window - 1) // QBLK)
    pad = n_prev * QBLK
    slab = pad + QBLK
    kp = jnp.pad(k, ((0, 0), (pad, 0), (0, 0), (0, 0)))
    vp = jnp.pad(v, ((0, 0), (pad, 0), (0, 0), (0, 0)))
    n_blk = S // QBLK
    qb = q.reshape(B, n_blk, QBLK, G, R, dh).transpose(1, 0, 2, 3, 4, 5)
    qi = jnp.arange(QBLK)[:, None]
    kj = jnp.arange(slab)[None, :]
    dist = pad + qi - kj
    in_band = (dist >= 0) & (dist < window)
    bias = rel_table.astype(jnp.float32)[rel_bucket(dist)]
    bias = bias.reshape(QBLK, slab, G, R).transpose(2, 3, 0, 1)
    sink = None if sinks is None else sinks.astype(jnp.float32).reshape(G, R, 1, 1)
    scale = dh ** -0.5

    def block(args):
        n, q_n = args
        start = n * QBLK
        k_n = lax.dynamic_slice_in_dim(kp, start, slab, axis=1)
        v_n = lax.dynamic_slice_in_dim(vp, start, slab, axis=1)
        s = jnp.einsum("bqgrd,bkgd->bgrqk", q_n, k_n).astype(jnp.float32) * scale + bias
        ok = in_band & (start - pad + kj >= 0)
        p = masked_softmax(s, ok, sink)
        o = jnp.einsum("bgrqk,bkgd->bqgrd", p.astype(v.dtype), v_n)
        return o.reshape(B, QBLK, H, dh)

    out = lax.map(block, (jnp.arange(n_blk), qb))
    return out.transpose(1, 0, 2, 3, 4).reshape(B, S, H, dh)


def sliding_sink_attention(h, w_in, w_out, sinks, rel_table):
    B, S, _ = h.shape
    qw, kw = N_HEADS * HEAD_DIM, A_KV_HEADS * HEAD_DIM
    q, k, v = jnp.split(h @ w_in, [qw, qw + kw], axis=-1)
    q = q.reshape(B, S, N_HEADS, HEAD_DIM)
    k = k.reshape(B, S, A_KV_HEADS, HEAD_DIM)
    v = v.reshape(B, S, A_KV_HEADS, HEAD_DIM)
    o = banded_attention(q, k, v, rel_table, A_WINDOW, sinks)
    return o.reshape(B, S, -1) @ w_out


def compress_kv(kv, pos, w1, w2):
    B, S, G, dh = kv.shape
    n_cmp = (S - CMP_LEN) // CMP_STRIDE + 1
    idx = jnp.arange(n_cmp)[:, None] * CMP_STRIDE + jnp.arange(CMP_LEN)[None, :]
    blocks = kv[:, idx] + pos[:, None, :]
    flat = blocks.transpose(0, 1, 3, 2, 4).reshape(B, n_cmp, G, CMP_LEN * dh)
    return jax.nn.gelu(flat @ w1) @ w2


def selected_block_attention(q, k, v, sel_idx, rel_table):
    B, S, G, R, dh = q.shape
    n_sel = sel_idx.shape[-1]
    n_slc = S // SLC_BLK
    kb = k.reshape(B, n_slc, SLC_BLK, G, dh).transpose(0, 3, 1, 2, 4)
    vb = v.reshape(B, n_slc, SLC_BLK, G, dh).transpose(0, 3, 1, 2, 4)
    table_g = rel_table.astype(jnp.float32).reshape(REL_BUCKETS, G, R).transpose(1, 0, 2)
    g_ix = jnp.arange(G)[:, None, None]
    n_q = S // B_QCHUNK
    n_keys = n_sel * SLC_BLK
    scale = dh ** -0.5

    def chunk(i):
        b, c = i // n_q, i % n_q
        start = c * B_QCHUNK
        q_c = lax.dynamic_slice_in_dim(q[b], start, B_QCHUNK, axis=0)
        idx = lax.dynamic_slice_in_dim(sel_idx[b], start, B_QCHUNK, axis=1)
        k_sel = kb[b][g_ix, idx].reshape(G, B_QCHUNK, n_keys, dh)
        v_sel = vb[b][g_ix, idx].reshape(G, B_QCHUNK, n_keys, dh)
        pos = (idx[..., None] * SLC_BLK + jnp.arange(SLC_BLK)).reshape(G, B_QCHUNK, n_keys)
        t = start + jnp.arange(B_QCHUNK)
        dist = t[None, :, None] - pos
        bias = table_g[g_ix, rel_bucket(dist)].transpose(0, 3, 1, 2)
        s = jnp.einsum("qgrd,gqkd->grqk", q_c, k_sel).astype(jnp.float32) * scale + bias
        p = masked_softmax(s, (dist >= 0)[:, None])
        o = jnp.einsum("grqk,gqkd->qgrd", p.astype(v.dtype), v_sel)
        return o.reshape(B_QCHUNK, G * R, dh)

    out = lax.map(chunk, jnp.arange(B * n_q))
    return out.reshape(B, S, G * R, dh)


def nsa_attention(h, w_in, w_out, cmp_pos, cmp_w1, cmp_w2, rel_table):
    B, S, _ = h.shape
    G, R, dh = B_KV_GROUPS, N_HEADS // B_KV_GROUPS, HEAD_DIM
    kvw = G * dh
    splits = np.cumsum([N_HEADS * dh] + [kvw] * 6).tolist()
    q, kc, vc, ks, vs, kw, vw, gate = jnp.split(h @ w_in, splits, axis=-1)
    q = q.reshape(B, S, G, R, dh)
    to_kv = lambda a: a.reshape(B, S, G, dh)
    scale = dh ** -0.5
    t = jnp.arange(S)

    kc_b = compress_kv(to_kv(kc), cmp_pos[0], cmp_w1[0], cmp_w2[0])
    vc_b = compress_kv(to_kv(vc), cmp_pos[1], cmp_w1[1], cmp_w2[1])
    n_cmp = kc_b.shape[1]
    cmp_end = jnp.arange(n_cmp) * CMP_STRIDE + CMP_LEN - 1
    s_cmp = jnp.einsum("bsgrd,bngd->bgrsn", q, kc_b).astype(jnp.float32) * scale
    p_cmp = masked_softmax(s_cmp, cmp_end[None, :] <= t[:, None])
    o_cmp = jnp.einsum("bgrsn,bngd->bsgrd", p_cmp.astype(vc_b.dtype), vc_b)

    n_slc = S // SLC_BLK
    n_sel = min(SLC_TOPK, n_slc)
    c_start = np.arange(n_cmp)[:, None] * CMP_STRIDE
    s_start = np.arange(n_slc)[None, :] * SLC_BLK
    overlap = ((c_start < s_start + SLC_BLK) & (c_start + CMP_LEN > s_start)).astype(np.float32)
    imp = jnp.einsum("bgrsn,nj->bgsj", p_cmp, jnp.asarray(overlap))
    cur = (t // SLC_BLK)[:, None]
    j = jnp.arange(n_slc)[None, :]
    forced = (j == 0) | (j == cur) | (j == cur - 1)
    imp = jnp.where(j > cur, NEG, jnp.where(forced, -NEG, imp))
    _, sel_idx = lax.top_k(imp, n_sel)
    o_slc = selected_block_attention(q, to_kv(ks), to_kv(vs), sel_idx, rel_table)

    o_win = banded_attention(q.reshape(B, S, N_HEADS, dh), to_kv(kw), to_kv(vw),
                             rel_table, B_WINDOW)

    g = jax.nn.sigmoid(gate.astype(jnp.float32)).reshape(B, S, N_HEADS, 3).astype(o_win.dtype)
    o = (g[..., 0:1] * o_cmp.reshape(B, S, N_HEADS, dh)
         + g[..., 1:2] * o_slc + g[..., 2:3] * o_win)
    return o.reshape(B, S, -1) @ w_out


def moba_attention(h, w_in, w_out, rel_table):
    B, S, _ = h.shape
    H, dh = N_HEADS, HEAD_DIM
    q, k, v = jnp.split(h @ w_in, 3, axis=-1)
    q = q.reshape(B, S, H, dh)
    k = k.reshape(B, S, H, dh)
    v = v.reshape(B, S, H, dh)
    n_blk = -(-S // MOBA_BLK)
    s_pad = n_blk * MOBA_BLK
    kb = jnp.pad(k, ((0, 0), (0, s_pad - S), (0, 0), (0, 0))).reshape(B, n_blk, MOBA_BLK, H, dh)
    vb = jnp.pad(v, ((0, 0), (0, s_pad - S), (0, 0), (0, 0))).reshape(B, n_blk, MOBA_BLK, H, dh)
    n_top = min(MOBA_TOPK, n_blk - 1)
    table = rel_table.astype(jnp.float32)
    table_h = table.T
    scale = dh ** -0.5
    t = jnp.arange(S)
    if n_top > 0:
        k_mean = jnp.mean(kb.astype(jnp.float32), axis=2).astype(q.dtype)
        gate = jnp.einsum("bshd,bnhd->bhsn", q, k_mean).astype(jnp.float32)
        past = jnp.arange(n_blk)[None, :] < (t // MOBA_BLK)[:, None]
        _, sel = lax.top_k(jnp.where(past, gate, NEG), n_top)
        kbh = kb.transpose(0, 3, 1, 2, 4)
        vbh = vb.transpose(0, 3, 1, 2, 4)
    h_ix = jnp.arange(H)[:, None, None]
    n_q = S // MOBA_QCHUNK

    def chunk(i):
        b, c = i // n_q, i % n_q
        start = c * MOBA_QCHUNK
        q_c = lax.dynamic_slice_in_dim(q[b], start, MOBA_QCHUNK, axis=0)
        tq = start + jnp.arange(MOBA_QCHUNK)
        own = start // MOBA_BLK
        k_own = lax.dynamic_index_in_dim(kb[b], own, axis=0, keepdims=False)
        v_own = lax.dynamic_index_in_dim(vb[b], own, axis=0, keepdims=False)
        d_own = tq[:, None] - (own * MOBA_BLK + jnp.arange(MOBA_BLK))[None, :]
        s_own = (jnp.einsum("qhd,khd->hqk", q_c, k_own).astype(jnp.float32) * scale
                 + table[rel_bucket(d_own)].transpose(2, 0, 1))
        ok_own = jnp.broadcast_to(d_own >= 0, s_own.shape)
        if n_top == 0:
            p = masked_softmax(s_own, ok_own)
            return jnp.einsum("hqk,khd->qhd", p.astype(v.dtype), v_own)
        idx = lax.dynamic_slice_in_dim(sel[b], start, MOBA_QCHUNK, axis=1)
        n_keys = n_top * MOBA_BLK
        k_sel = kbh[b][h_ix, idx].reshape(H, MOBA_QCHUNK, n_keys, dh)
        v_sel = vbh[b][h_ix, idx].reshape(H, MOBA_QCHUNK, n_keys, dh)
        pos = (idx[..., None] * MOBA_BLK + jnp.arange(MOBA_BLK)).reshape(H, MOBA_QCHUNK, n_keys)
        d_sel = tq[None, :, None] - pos
        s_sel = (jnp.einsum("qhd,hqkd->hqk", q_c, k_sel).astype(jnp.float32) * scale
                 + table_h[h_ix, rel_bucket(d_sel)])
        ok_sel = jnp.broadcast_to((idx < own)[..., None],
                                  (H, MOBA_QCHUNK, n_top, MOBA_BLK)).reshape(H, MOBA_QCHUNK, n_keys)
        p = masked_softmax(jnp.concatenate([s_own, s_sel], axis=-1),
                           jnp.concatenate([ok_own, ok_sel], axis=-1)).astype(v.dtype)
        return (jnp.einsum("hqk,khd->qhd", p[..., :MOBA_BLK], v_own)
                + jnp.einsum("hqk,hqkd->qhd", p[..., MOBA_BLK:], v_sel))

    out = lax.map(chunk, jnp.arange(B * n_q))
    return out.reshape(B, S, H * dh) @ w_out


def setup_inputs(seed: int = 0) -> dict:
    key = jax.random.key(seed)
    ks = jax.random.split(key, 16)
    f32 = jnp.float32
    hd = N_HEADS * HEAD_DIM

    def nrm(k, shape, fan_in):
        return jax.random.normal(k, shape, f32) * fan_in ** -0.5

    return {
        "x": jax.random.normal(ks[0], (BATCH, SEQ, D_MODEL), f32),
        "rel_bias": 0.5 * jax.random.normal(ks[1], (REL_BUCKETS, N_HEADS), f32),
        "norm_g": 1.0 + 0.05 * jax.random.normal(ks[2], (DEPTH, 6, D_MODEL), f32),
        "ffn_w_in": nrm(ks[3], (DEPTH, 2, D_MODEL, 2 * D_FF), D_MODEL),
        "ffn_w_out": nrm(ks[4], (DEPTH, 2, D_FF, D_MODEL), D_FF),
        "a_w_in": nrm(ks[5], (N_A_LAYERS, D_MODEL, A_IN), D_MODEL),
        "a_w_out": nrm(ks[6], (N_A_LAYERS, hd, D_MODEL), hd),
        "a_sinks": jax.random.normal(ks[7], (N_A_LAYERS, N_HEADS), f32),
        "b_w_in": nrm(ks[8], (N_B_LAYERS, D_MODEL, B_IN), D_MODEL),
        "b_w_out": nrm(ks[9], (N_B_LAYERS, hd, D_MODEL), hd),
        "b_cmp_pos": 0.5 * jax.random.normal(ks[10], (N_B_LAYERS, 2, CMP_LEN, HEAD_DIM), f32),
        "b_cmp_w1": nrm(ks[11], (N_B_LAYERS, 2, CMP_LEN * HEAD_DIM, CMP_HIDDEN), CMP_LEN * HEAD_DIM),
        "b_cmp_w2": nrm(ks[12], (N_B_LAYERS, 2, CMP_HIDDEN, HEAD_DIM), CMP_HIDDEN),
        "c_w_in": nrm(ks[13], (N_C_LAYERS, D_MODEL, C_IN), D_MODEL),
        "c_w_out": nrm(ks[14], (N_C_LAYERS, hd, D_MODEL), hd),
    }


def reference(x, rel_bias, norm_g, ffn_w_in, ffn_w_out, a_w_in, a_w_out, a_sinks,
              b_w_in, b_w_out, b_cmp_pos, b_cmp_w1, b_cmp_w2, c_w_in, c_w_out):
    for i in range(DEPTH):
        g = norm_g[i]
        x = x + 0.5 * rms_norm(swiglu(rms_norm(x, g[0]), ffn_w_in[i, 0], ffn_w_out[i, 0]), g[1])
        hm = rms_norm(x, g[2])
        kind, j = i % N_MIXERS, i // N_MIXERS
        if kind == 0:
            m = sliding_sink_attention(hm, a_w_in[j], a_w_out[j], a_sinks[j], rel_bias)
        elif kind == 1:
            m = nsa_attention(hm, b_w_in[j], b_w_out[j], b_cmp_pos[j], b_cmp_w1[j],
                              b_cmp_w2[j], rel_bias)
        else:
            m = moba_attention(hm, c_w_in[j], c_w_out[j], rel_bias)
        x = x + rms_norm(m, g[3])
        x = x + 0.5 * rms_norm(swiglu(rms_norm(x, g[4]), ffn_w_in[i, 1], ffn_w_out[i, 1]), g[5])
    return x
```

```python
import math
from contextlib import ExitStack
import numpy as np
import concourse.bass as bass
import concourse.mybir as mybir
from concourse.bass_utils import run_bass_kernel_spmd

F32 = mybir.dt.float32
BF16 = mybir.dt.bfloat16
AF = mybir.ActivationFunctionType
ALU = mybir.AluOpType
AX = mybir.AxisListType

D = 1024
FF = 2816
S = 2048
NH = 16
DH = 64
EPS = 1e-6
MT = 1024
ARENA_WORDS = 50 * 1024
EBW = 1536
EBC = 1024
REL_U = EBW + 127

COMPUTE = ("tensor", "vector", "scalar", "gpsimd")
QUEUES = ("sync", "gpsimd", "scalar")
NPOOL = 12


class Em:
    def __init__(self, nc, es):
        self.nc = nc
        self.es = es
        self.ops = []

    def op(self, eng, fn, r=(), w=()):
        self.ops.append((eng, fn, tuple(r), tuple(w), 0))

    def dma(self, eng, fn, r=(), w=(), n=1):
        self.ops.append((eng, fn, tuple(r), tuple(w), n))

    def barrier(self):
        self.ops.append(("barrier", None, (), (), 0))

    def emit(self):
        nc, es = self.nc, self.es
        sems = []

        def newsem(name):
            sems.append(es.enter_context(nc.semaphore(name)))
            return len(sems) - 1

        esem = {e: newsem("c_" + e) for e in COMPUTE}
        pool = {q: [[newsem("d_%s%d" % (q, i)), 0] for i in range(NPOOL)] for q in QUEUES}
        pnext = {q: 0 for q in QUEUES}
        count = {e: 0 for e in COMPUTE}
        engs = ("tensor", "vector", "scalar", "gpsimd", "sync")
        known = {e: {} for e in engs}
        last_w = {}
        readers = {}
        streams = {e: [] for e in engs}

        def all_tokens():
            toks = []
            for q in QUEUES:
                for s, v in pool[q]:
                    if v > 0:
                        toks.append((s, v))
            for e in COMPUTE:
                if count[e] > 0:
                    toks.append((esem[e], count[e]))
            return toks

        for (eng, fn, r, w, ndma) in self.ops:
            if eng == "barrier":
                toks = all_tokens()
                for e in engs:
                    for s, v in toks:
                        if e == "tensor" and s == esem["tensor"]:
                            continue
                        if known[e].get(s, 0) >= v:
                            continue
                        streams[e].append(("wait", s, v))
                        known[e][s] = v
                last_w = {}
                readers = {}
                continue
            need = {}

            def add(tok):
                s, v = tok
                if need.get(s, 0) < v:
                    need[s] = v

            for k in r:
                if k in last_w:
                    add(last_w[k])
            for k in w:
                if k in last_w:
                    add(last_w[k])
                for s, v in readers.get(k, {}).items():
                    add((s, v))
            slot = None
            if ndma:
                slot = pool[eng][pnext[eng]]
                pnext[eng] = (pnext[eng] + 1) % NPOOL
                if slot[1] > 0:
                    add((slot[0], slot[1]))
            for s, v in need.items():
                if eng == "tensor" and s == esem["tensor"]:
                    continue
                if known[eng].get(s, 0) >= v:
                    continue
                streams[eng].append(("wait", s, v))
                known[eng][s] = v
            if ndma:
                slot[1] += 16 * ndma
                tok = (slot[0], slot[1])
            else:
                count[eng] += 1
                tok = (esem[eng], count[eng])
            streams[eng].append(("op", fn, tok, ndma))
            for k in w:
                last_w[k] = tok
                readers[k] = {}
            for k in r:
                d = readers.setdefault(k, {})
                if d.get(tok[0], 0) < tok[1]:
                    d[tok[0]] = tok[1]
        for s, v in all_tokens():
            if known["sync"].get(s, 0) < v:
                streams["sync"].append(("wait", s, v))
        self.n_instr = {e: len(streams[e]) for e in engs}
        self.streams = streams
        self.esem = esem

        def run(engobj, lst):
            for it in lst:
                if it[0] == "wait":
                    engobj.wait_ge(sems[it[1]], it[2])
                else:
                    _, fn, tok, ndma = it
                    res = fn(engobj)
                    if ndma:
                        assert len(res) == ndma, (len(res), ndma)
                        for ins in res:
                            ins.then_inc(sems[tok[0]], 16)
                    else:
                        res.then_inc(sems[tok[0]], 1)

        with nc.Block() as block:
            @block.tensor
            def _(e):
                run(e, streams["tensor"])

            @block.vector
            def _(e):
                run(e, streams["vector"])

            @block.scalar
            def _(e):
                run(e, streams["scalar"])

            @block.gpsimd
            def _(e):
                run(e, streams["gpsimd"])

            @block.sync
            def _(e):
                run(e, streams["sync"])


class Arena:
    def __init__(self, t):
        self.t = t
        self.off = 0

    def f32(self, n):
        v = self.t[:, self.off:self.off + n]
        self.off += n
        assert self.off <= ARENA_WORDS, self.off
        return v

    def bf16(self, n):
        words = (n + 1) // 2
        v = self.t[:, self.off:self.off + words].bitcast(BF16)
        self.off += words
        assert self.off <= ARENA_WORDS, self.off
        return v[:, 0:n]


class Builder:
    def __init__(self, nseq, plan):
        self.nseq = nseq
        self.ntok = nseq * S
        self.plan = plan
        self.nc = bass.Bass("TRN2", target_bir_lowering=False)
        self.dram = {}
        self.ext_inputs = []

    def din(self, name, shape, dt=F32):
        if name not in self.dram:
            self.dram[name] = self.nc.dram_tensor(name, list(shape), dt, kind="ExternalInput")
            self.ext_inputs.append(name)
        return self.dram[name]

    def dscratch(self, name, shape, dt=F32):
        if name not in self.dram:
            self.dram[name] = self.nc.dram_tensor(name, list(shape), dt, kind="Internal")
        return self.dram[name]

    def MM(self, out, lhsT, rhs, start, stop, r, w, skip=False):
        if skip:
            self.em.op("tensor", lambda e: e.matmul(out, lhsT=lhsT, rhs=rhs, start=start, stop=stop,
                                                    skip_group_check=True), r, w)
        else:
            self.em.op("tensor", lambda e: e.matmul(out, lhsT=lhsT, rhs=rhs, start=start, stop=stop), r, w)

    def TR(self, out, in_, r, w):
        k = in_.shape[0]
        ident = self.ident[0:k, 0:k]
        self.em.op("tensor", lambda e: e.transpose(out=out, in_=in_, identity=ident), tuple(r) + ("ident",), w)

    def TRF(self, out, in_, r, w):
        k = in_.shape[0]
        ident = self.identf[0:k, 0:k]
        self.em.op("tensor", lambda e: e.transpose(out=out, in_=in_, identity=ident), tuple(r) + ("identf",), w)

    def ACT(self, out, in_, func, r, w, **kw):
        self.em.op("scalar", lambda e: e.activation(out=out, in_=in_, func=func, **kw), r, w)

    def TT(self, eng, out, in0, in1, op, r, w):
        self.em.op(eng, lambda e: e.tensor_tensor(out=out, in0=in0, in1=in1, op=op), r, w)

    def TS(self, eng, out, in0, s1, s2, op0, op1, r, w):
        if s2 is None:
            self.em.op(eng, lambda e: e.tensor_scalar(out=out, in0=in0, scalar1=s1, scalar2=None, op0=op0), r, w)
        else:
            self.em.op(eng, lambda e: e.tensor_scalar(out=out, in0=in0, scalar1=s1, scalar2=s2, op0=op0, op1=op1), r, w)

    def STT(self, eng, out, in0, scalar, in1, op0, op1, r, w):
        self.em.op(eng, lambda e: e.scalar_tensor_tensor(out=out, in0=in0, scalar=scalar, in1=in1, op0=op0, op1=op1), r, w)

    def CP(self, eng, out, in_, r, w):
        if eng == "scalar":
            self.em.op(eng, lambda e: e.copy(out=out, in_=in_), r, w)
        else:
            self.em.op(eng, lambda e: e.tensor_copy(out=out, in_=in_), r, w)

    def RECIP(self, out, in_, r, w):
        self.em.op("vector", lambda e: e.reciprocal(out=out, in_=in_), r, w)

    def MEMSET(self, eng, ap, val, w):
        self.em.op(eng, lambda e: e.memset(ap, val), (), w)

    def DMA(self, eng, out, in_, r, w):
        self.em.dma(eng, lambda e: [e.dma_start(out=out, in_=in_)], r, w, n=1)

    def RSTD(self, ss, tmp, rs, key):
        self.TS("vector", tmp, ss, 1.0 / D, EPS, ALU.mult, ALU.add, [key], [key])
        self.ACT(tmp, tmp, AF.Sqrt, [key], [key])
        self.RECIP(rs, tmp, [key], [key])

    def build(self):
        nc = self.nc
        with ExitStack() as es:
            self.es = es
            self.em = Em(nc, es)
            arena_t = es.enter_context(nc.sbuf_tensor("arena", [128, ARENA_WORDS], F32))
            self.psum = es.enter_context(nc.psum_tensor("psum", [128, 4096], F32))
            self.A = Arena(arena_t)
            self.x_in = self.din("x", [self.ntok, D])
            self.y = self.nc.dram_tensor("y", [self.ntok, D], F32, kind="ExternalOutput")
            self.norm_g = self.din("norm_g", [4, 6, D])
            self.setup_consts()
            self.base_off = self.A.off
            cur = self.x_in
            for ph in self.plan:
                self.A.off = self.base_off
                self.em.barrier()
                if ph[0] == "ffn":
                    self.ffn(ph[1], ph[2], cur, self.y)
                    cur = self.y
                elif ph[0] == "copy":
                    self.copy_phase(cur, self.y)
                    cur = self.y
                elif ph[0] == "rel":
                    self.rel_phase()
                elif ph[0] == "mix":
                    self.mixer(ph[1], cur, self.y)
                    cur = self.y
                else:
                    raise ValueError(ph)
            self.em.emit()
        return nc

    def pb(self, i, n=1):
        return self.psum[:, 512 * i:512 * (i + n)]

    def setup_consts(self):
        A = self.A
        self.ident = A.bf16(128)
        identf = A.f32(128)
        self.identf = identf
        self.MEMSET("gpsimd", identf, 0.0, ["identf"])
        self.em.op("gpsimd", lambda e: e.affine_select(out=identf, in_=identf, pattern=[[-1, 128]],
                   compare_op=ALU.not_equal, fill=1.0, base=0, channel_multiplier=1), ["identf"], ["identf"])
        self.CP("vector", self.ident, identf, ["identf"], ["ident"])

    def copy_phase(self, src, dst):
        A = self.A
        bufs = [A.f32(D) for _ in range(2)]
        for t in range(self.ntok // 128):
            b = t % 2
            self.DMA("sync", bufs[b], src.ap()[t * 128:(t + 1) * 128, :], [], ["cb%d" % b])
            self.DMA("sync", dst.ap()[t * 128:(t + 1) * 128, :], bufs[b], ["cb%d" % b], [])

    def ffn(self, layer, which, src, dst):
        A = self.A
        em = self.em
        w_in = self.din("ffn_w_in", [4, 2, D, 2 * FF]).ap()[layer, which]
        w_out = self.din("ffn_w_out", [4, 2, FF, D]).ap()[layer, which]
        gi = 0 if which == 0 else 4
        g_in = A.f32(D)
        g_out = A.f32(D)
        self.DMA("sync", g_in, self.norm_g.ap()[layer, gi:gi + 1, :].broadcast_to([128, D]), [], ["g_in"])
        self.DMA("sync", g_out, self.norm_g.ap()[layer, gi + 1:gi + 2, :].broadcast_to([128, D]), [], ["g_out"])
        self.TS("vector", g_out, g_out, 0.5, None, ALU.mult, None, ["g_out"], ["g_out"])
        xa = [A.f32(D) for _ in range(2)]
        junk = A.f32(D)
        hn = [A.bf16(D) for _ in range(2)]
        hT = A.bf16(8 * MT).rearrange("p (k t) -> p k t", k=8)
        aT = A.bf16(22 * MT).rearrange("p (c t) -> p c t", c=22)
        wi = [A.bf16(8 * 1024).rearrange("p (k f) -> p k f", k=8) for _ in range(2)]
        wo = A.bf16(22 * D).rearrange("p (c d) -> p c d", c=22)
        sg = [A.f32(512) for _ in range(2)]
        st = A.f32(64)
        xe = [A.f32(D) for _ in range(2)]
        tmp = [A.f32(D) for _ in range(2)]
        xo = [A.f32(D) for _ in range(2)]
        w_in_v = w_in.rearrange("(k p) f -> p k f", p=128)
        w_out_v = w_out.rearrange("(c p) d -> p c d", p=128)
        nmt = self.ntok // MT
        ngrp = 6
        grp_chunks = [list(range(g * 4, min(22, g * 4 + 4))) for g in range(ngrp)]
        stream = [(m, g) for m in range(nmt) for g in range(ngrp)]

        def load_group(idx):
            m, g = stream[idx]
            b = idx % 2
            ch = grp_chunks[g]
            ncol = 128 * len(ch)
            c0 = ch[0] * 128
            em.dma("gpsimd", lambda e: [
                e.dma_start(out=wi[b][:, :, 0:ncol], in_=w_in_v[:, :, c0:c0 + ncol]),
                e.dma_start(out=wi[b][:, :, 512:512 + ncol], in_=w_in_v[:, :, FF + c0:FF + c0 + ncol]),
            ], [], [("wi", b)], n=2)

        pst = self.pb(7).bitcast(BF16)
        load_group(0)
        for m in range(nmt):
            for s in range(8):
                t0 = m * MT + s * 128
                b = s % 2
                kx = "xa%d" % b
                ks = "st%d" % s
                self.DMA("sync", xa[b], src.ap()[t0:t0 + 128, :], [], [kx])
                self.MEMSET("vector", st[:, s:s + 1], 0.0, [ks])
                self.ACT(junk, xa[b], AF.Square, [kx, ks], ["junk", ks], accum_out=st[:, s:s + 1])
                self.RSTD(st[:, s:s + 1], st[:, 8 + s:9 + s], st[:, 16 + s:17 + s], ks)
                self.STT("vector", hn[b], xa[b], st[:, 16 + s:17 + s], g_in, ALU.mult, ALU.mult,
                         [kx, ks, "g_in"], ["hn%d" % b])
                for kc in range(8):
                    self.TR(pst[:, kc * 128:(kc + 1) * 128], hn[b][:, kc * 128:(kc + 1) * 128],
                            ["hn%d" % b], [("ps", 7)])
                self.CP("scalar", hT[:, :, s * 128:(s + 1) * 128],
                        pst.rearrange("p (k t) -> p k t", k=8), [("ps", 7)], [("hT", s)])
            if getattr(self, "dbg", "") == "B":
                continue
            for g in range(ngrp):
                idx = m * ngrp + g
                if idx + 1 < len(stream):
                    load_group(idx + 1)
                b = idx % 2
                for ci, c in enumerate(grp_chunks[g]):
                    self.DMA("gpsimd", wo[:, c, :], w_out_v[:, c, :], [], [("wo", c)])
                    for tt in range(2):
                        pg = self.pb(2 * tt)
                        pu = self.pb(2 * tt + 1)
                        hkeys = [("hT", 4 * tt + i) for i in range(4)]
                        for kc in range(8):
                            self.MM(pg, wi[b][:, kc, ci * 128:(ci + 1) * 128], hT[:, kc, tt * 512:(tt + 1) * 512],
                                    kc == 0, kc == 7, [("wi", b)] + hkeys, [("ps", 2 * tt)])
                        for kc in range(8):
                            self.MM(pu, wi[b][:, kc, 512 + ci * 128:512 + (ci + 1) * 128],
                                    hT[:, kc, tt * 512:(tt + 1) * 512],
                                    kc == 0, kc == 7, [("wi", b)] + hkeys, [("ps", 2 * tt + 1)])
                        self.ACT(sg[tt], pg, AF.Silu, [("ps", 2 * tt)], ["sg%d" % tt])
                        self.TT("vector", aT[:, c, tt * 512:(tt + 1) * 512], sg[tt], pu, ALU.mult,
                                ["sg%d" % tt, ("ps", 2 * tt + 1)], [("aT", c, tt)])
            if getattr(self, "dbg", "") == "BC":
                continue
            for s in range(8):
                t0 = m * MT + s * 128
                b = s % 2
                tt = s // 4
                pm = self.pb(4 + 2 * b, 2)
                kpm = ("ps", 4 + 2 * b)
                for nh in range(2):
                    for c in range(22):
                        self.MM(pm[:, nh * 512:(nh + 1) * 512], aT[:, c, s * 128:(s + 1) * 128],
                                wo[:, c, nh * 512:(nh + 1) * 512], c == 0, c == 21,
                                [("aT", c, tt), ("wo", c)], [kpm, ("ps", 5 + 2 * b)])
                ks = "st2%d" % s
                self.DMA("sync", xe[b], src.ap()[t0:t0 + 128, :], [], ["xe%d" % b])
                self.MEMSET("vector", st[:, 24 + s:25 + s], 0.0, [ks])
                for nh in range(2):
                    self.CP("vector", tmp[b][:, nh * 512:(nh + 1) * 512], pm[:, nh * 512:(nh + 1) * 512], [kpm], ["tmp%d" % b])
                self.ACT(junk, tmp[b], AF.Square, ["tmp%d" % b, ks], ["junk", ks], accum_out=st[:, 24 + s:25 + s])
                self.RSTD(st[:, 24 + s:25 + s], st[:, 32 + s:33 + s], st[:, 40 + s:41 + s], ks)
                self.STT("vector", xo[b], tmp[b], st[:, 40 + s:41 + s], g_out, ALU.mult, ALU.mult,
                         ["tmp%d" % b, ks, "g_out"], ["xo%d" % b])
                self.TT("vector", xo[b], xo[b], xe[b], ALU.add, ["xo%d" % b, "xe%d" % b], ["xo%d" % b])
                self.DMA("sync", dst.ap()[t0:t0 + 128, :], xo[b], ["xo%d" % b], [])


    def norm_T(self, src, t0, s, g_in, xa, junk, hn, st, hT, pst):
        b = s % 2
        kx = "xa%d" % b
        ks = "st%d" % s
        self.DMA("sync", xa[b], src.ap()[t0:t0 + 128, :], [], [kx])
        self.MEMSET("vector", st[:, s:s + 1], 0.0, [ks])
        self.ACT(junk, xa[b], AF.Square, [kx, ks], ["junk", ks], accum_out=st[:, s:s + 1])
        self.RSTD(st[:, s:s + 1], st[:, 8 + s:9 + s], st[:, 16 + s:17 + s], ks)
        self.STT("vector", hn[b], xa[b], st[:, 16 + s:17 + s], g_in, ALU.mult, ALU.mult,
                 [kx, ks, "g_in"], ["hn%d" % b])
        for kc in range(8):
            self.TR(pst[:, kc * 128:(kc + 1) * 128], hn[b][:, kc * 128:(kc + 1) * 128],
                    ["hn%d" % b], [("ps", 7)])
        self.CP("scalar", hT[:, :, s * 128:(s + 1) * 128],
                pst.rearrange("p (k t) -> p k t", k=8), [("ps", 7)], [("hT", s)])

    def rel_phase(self):
        A = self.A
        U = REL_U
        oh = self.din("oh", [3, 33, U])
        rel = self.din("rel_bias", [32, NH])
        Z = self.dscratch("Z", [3, NH, 128, U], BF16)
        tab = A.f32(NH)[0:33, :]
        self.MEMSET("vector", tab, -30000.0, ["tab"])
        self.DMA("sync", tab[0:32, :], rel.ap(), [], ["tab"])
        oht = A.f32(U)[0:33, :]
        Gs = A.bf16(U)[0:16, :]
        for ty in range(3):
            self.DMA("sync", oht, oh.ap()[ty], [], ["oht"])
            for c0 in range(0, U, 512):
                n = min(512, U - c0)
                bank = (c0 // 512) % 4
                self.MM(self.pb(bank)[0:16, 0:n], tab, oht[:, c0:c0 + n], True, True, ["tab", "oht"], [("ps", bank)])
                self.ACT(Gs[:, c0:c0 + n], self.pb(bank)[0:16, 0:n], AF.Exp, [("ps", bank)], ["Gs"])
            self.DMA("sync", Z.ap()[ty], Gs.unsqueeze(1).broadcast_to([NH, 128, U]), ["Gs"], [])

    def eb_ap(self, ty, h):
        Z = self.dram["Z"]
        U = REL_U
        return bass.AP(tensor=Z, offset=((ty * NH + h) * 128) * U + 127, ap=[[U - 1, 128], [1, EBW]])

    def proj(self, src, layer, w_ap, ncols, fm_groups, tm_chunks, FT, TM):
        A = self.A
        em = self.em
        g_in = A.f32(D)
        self.DMA("sync", g_in, self.norm_g.ap()[layer, 2:3, :].broadcast_to([128, D]), [], ["g_in"])
        w_sb = A.bf16(8 * ncols).rearrange("p (k f) -> p k f", k=8)
        w_v = w_ap.rearrange("(k p) f -> p k f", p=128)
        for c0 in range(0, ncols, 512):
            c1 = min(ncols, c0 + 512)
            self.DMA("gpsimd", w_sb[:, :, c0:c1], w_v[:, :, c0:c1], [], [("w", c0 // 512)])

        def wkeys(c, n):
            return [("w", i) for i in range(c // 512, (c + n - 1) // 512 + 1)]

        xa = [A.f32(D) for _ in range(2)]
        junk = A.f32(D)
        hn = [A.bf16(D) for _ in range(2)]
        hT = A.bf16(8 * 512).rearrange("p (k t) -> p k t", k=8)
        st = A.f32(64)
        stg = [A.bf16(512) for _ in range(3)]
        stg2 = [A.bf16(512) for _ in range(3)]
        pst = self.pb(7).bitcast(BF16)
        cnt = 0
        for mt in range(self.ntok // 512):
            t0 = mt * 512
            for s in range(4):
                self.norm_T(src, t0 + s * 128, s, g_in, xa, junk, hn, st, hT, pst)
            hkeys = [("hT", i) for i in range(4)]
            for gi, (wc, frow) in enumerate(fm_groups):
                bank = gi % 3
                b3 = gi % 3
                for kc in range(8):
                    self.MM(self.pb(bank), w_sb[:, kc, wc:wc + 128], hT[:, kc, :], kc == 0, kc == 7,
                            wkeys(wc, 128) + hkeys, [("ps", bank)])
                self.CP("scalar" if gi % 2 == 0 else "vector", stg[b3], self.pb(bank), [("ps", bank)], ["stg%d" % b3])
                self.DMA("sync", FT.ap()[frow:frow + 128, t0:t0 + 512], stg[b3], ["stg%d" % b3], [])
            for s in range(4):
                for (wc, n, tcol) in tm_chunks:
                    bank = 3 + cnt % 3
                    b3 = cnt % 3
                    cnt += 1
                    for kc in range(8):
                        self.MM(self.pb(bank)[:, 0:n], hT[:, kc, s * 128:(s + 1) * 128], w_sb[:, kc, wc:wc + n],
                                kc == 0, kc == 7, wkeys(wc, n) + [("hT", s)], [("ps", bank)])
                    self.CP("vector" if cnt % 2 == 0 else "scalar", stg2[b3][:, 0:n], self.pb(bank)[:, 0:n],
                            [("ps", bank)], ["stg2%d" % b3])
                    self.DMA("sync", TM.ap()[t0 + s * 128:t0 + (s + 1) * 128, tcol:tcol + n], stg2[b3][:, 0:n],
                             ["stg2%d" % b3], [])

    def attend(self, qT, kq, kT, kk, vext, kv, EB, keb, window, fin, mask=None):
        E = self.E
        PT = self.PT
        LA = 2
        flat = []
        rounds = {}
        for m in range(4):
            q0 = 512 * m
            tiles = []
            for kt in range(16):
                j0 = max(0, 128 * kt - q0)
                j1 = 512 if window is None else min(512, 128 * kt + 127 + window - q0)
                j1 = ((j1 + 127) // 128) * 128
                if j1 > j0:
                    tiles.append((kt, j0, j1))
            last = {}
            for (kt, j0, j1) in tiles:
                for i in range(j0 // 128, j1 // 128):
                    last[i] = kt
            ab = 3 + self.acc_cnt % 2
            self.acc_cnt += 1
            rounds[m] = dict(last=last, ab=ab, opened=False, n=len(tiles), done=0)
            for (kt, j0, j1) in tiles:
                flat.append(dict(m=m, kt=kt, j0=j0, j1=j1))

        def stage1(t):
            m, kt, j0, j1 = t["m"], t["kt"], t["j0"], t["j1"]
            q0 = 512 * m
            n = j1 - j0
            sb = self.st_cnt % 3
            self.st_cnt += 1
            t["sb"] = sb
            ps = self.pb(sb)[:, 0:n]
            kps = ("ps", sb)
            self.MM(ps, kT[:, kt * 128:(kt + 1) * 128], qT[:, q0 + j0:q0 + j1], True, mask is None, [kk, kq], [kps])
            if mask is not None:
                MnegT, kmask, rowfn = mask
                self.MM(ps, rowfn(kt), MnegT[:, q0 + j0:q0 + j1], False, True, [kmask, "ident", "Esel"], [kps])
            self.ACT(E[sb][:, 0:n], ps, AF.Exp, [kps], ["E%d" % sb], scale=0.125)
            c0 = min(q0 + j0 - 128 * kt, EBC)
            self.TT("vector", PT[sb][:, 0:n], E[sb][:, 0:n], EB[:, c0:c0 + n], ALU.mult, ["E%d" % sb, keb], ["PT%d" % sb])

        def stage2(t):
            m, kt, j0, j1, sb = t["m"], t["kt"], t["j0"], t["j1"], t["sb"]
            rd = rounds[m]
            n = j1 - j0
            oT = self.pb(rd["ab"])[0:vext.shape[2], :]
            kacc = ("ps", rd["ab"])
            self.MM(oT[:, j0:j1], vext[:, kt, :], PT[sb][:, 0:n], not rd["opened"], True, ["PT%d" % sb, kv], [kacc], skip=True)
            rd["opened"] = True
            rd["done"] += 1
            if rd["done"] == rd["n"]:
                ob = self.t_cnt % 2
                tbk = 5 + self.t_cnt % 2
                self.t_cnt += 1
                oTs = self.oTs[ob][0:65, :]
                self.CP("scalar", oTs, oT[0:65, :], [kacc], ["oTs%d" % ob])
                T = self.pb(tbk).rearrange("p (i c) -> p i c", i=4)
                for i in range(4):
                    self.TRF(T[:, i, 0:65], oTs[:, i * 128:(i + 1) * 128], ["oTs%d" % ob], [("ps", tbk)])
                fin(m, T, ("ps", tbk))

        for _ in range(getattr(self, "warm", 0)):
            self.MM(self.pb(7), self.ident, self.wo_c[:, 0, 0:512], True, True, ["ident", "wo"], [("ps", 7)])
        for ti in range(len(flat) + LA):
            if ti < len(flat):
                stage1(flat[ti])
            if ti - LA >= 0:
                stage2(flat[ti - LA])

    def outproj(self, seq, O_sb, kO, wo, g_out, src, dst, bufs):
        OT, junk, tmp, xe, xo, st = bufs
        pst = self.pb(7).bitcast(BF16)
        for s in range(16):
            if getattr(self, "dbg", "") == "OUT1" and s >= 1:
                break
            t0 = seq * S + s * 128
            b = s % 2
            for kc in range(8):
                self.TR(pst[:, kc * 128:(kc + 1) * 128], O_sb[:, s, kc * 128:(kc + 1) * 128], [kO], [("ps", 7)])
            self.CP("scalar", OT[b], pst, [("ps", 7)], ["OT%d" % b])
            pm = self.pb(5, 2) if False else None
            banks = (5, 6)
            for nh in range(2):
                for kc in range(8):
                    self.MM(self.pb(banks[nh]), OT[b][:, kc * 128:(kc + 1) * 128], wo[:, kc, nh * 512:(nh + 1) * 512],
                            kc == 0, kc == 7, ["OT%d" % b, "wo"], [("ps", banks[nh])])
            if getattr(self, "dbg2", "") == "a":
                continue
            ks = "ost%d" % b
            self.DMA("sync", xe[b], src.ap()[t0:t0 + 128, :], [], ["xe%d" % b])
            self.MEMSET("vector", st[:, 8 * b:8 * b + 1], 0.0, [ks])
            for nh in range(2):
                self.CP("vector", tmp[b][:, nh * 512:(nh + 1) * 512], self.pb(banks[nh]), [("ps", banks[nh])], ["tmp%d" % b])
            self.ACT(junk, tmp[b], AF.Square, ["tmp%d" % b, ks], ["junk", ks], accum_out=st[:, 8 * b:8 * b + 1])
            self.RSTD(st[:, 8 * b:8 * b + 1], st[:, 8 * b + 2:8 * b + 3], st[:, 8 * b + 3:8 * b + 4], ks)
            self.STT("vector", xo[b], tmp[b], st[:, 8 * b + 3:8 * b + 4], g_out, ALU.mult, ALU.mult,
                     ["tmp%d" % b, ks, "g_out"], ["xo%d" % b])
            self.TT("vector", xo[b], xo[b], xe[b], ALU.add, ["xo%d" % b, "xe%d" % b], ["xo%d" % b])
            self.DMA("sync", dst.ap()[t0:t0 + 128, :], xo[b], ["xo%d" % b], [])

    def attn_common(self, layer, w_out_ap):
        A = self.A
        g_out = A.f32(D)
        self.DMA("sync", g_out, self.norm_g.ap()[layer, 3:4, :].broadcast_to([128, D]), [], ["g_out"])
        wo = A.bf16(8 * D).rearrange("p (k d) -> p k d", k=8)
        self.wo_c = wo
        self.DMA("gpsimd", wo, w_out_ap.rearrange("(k p) d -> p k d", p=128), [], ["wo"])
        O_sb = A.bf16(16 * D).rearrange("p (s d) -> p s d", s=16)
        self.E = [A.bf16(512) for _ in range(3)]
        self.oTs = [A.f32(512) for _ in range(2)]
        self.t_cnt = 0
        self.PT = [A.bf16(512) for _ in range(3)]
        OT = [A.bf16(D) for _ in range(2)]
        junk = A.f32(D)
        tmp = [A.f32(D) for _ in range(2)]
        xe = [A.f32(D) for _ in range(2)]
        xo = [A.f32(D) for _ in range(2)]
        st = A.f32(16)
        self.acc_cnt = 0
        self.st_cnt = 0
        return g_out, wo, O_sb, (OT, junk, tmp, xe, xo, st)

    def mixer(self, layer, src, dst):
        kind, j = layer % 3, layer // 3
        FT = self.dscratch("FT", [2048, self.ntok], BF16)
        TM = self.dscratch("TM", [self.ntok, 1024], BF16)
        if kind == 0:
            self.mixer_A(layer, j, src, dst, FT, TM)
        elif kind == 2:
            self.mixer_C(layer, j, src, dst, FT, TM)
        else:
            self.mixer_B(layer, j, src, dst, FT, TM)

    def mixer_A(self, layer, j, src, dst, FT, TM):
        A = self.A
        w_in = self.din("a_w_in", [2, D, 1280]).ap()[j]
        w_out = self.din("a_w_out", [2, D, D]).ap()[j]
        sinks = self.din("a_sinks", [2, NH]).ap()
        fm = [(g * 128, g * 128) for g in range(9)]
        tmc = [(1152, 128, 0)]
        self.proj(src, layer, w_in, 1280, fm, tmc, FT, TM)
        if getattr(self, "dbg", "") == "P1":
            return
        self.em.barrier()
        A.off = self.base_off
        g_out, wo, O_sb, bufs = self.attn_common(layer, w_out)
        esink = A.f32(NH)
        self.DMA("sync", esink, sinks[j:j + 1, :].broadcast_to([128, NH]), [], ["esink"])
        self.ACT(esink, esink, AF.Exp, ["esink"], ["esink"])
        qTf = [A.bf16(S) for _ in range(2)]
        kTf = [A.bf16(S) for _ in range(2)]
        qT = [t[0:64, :] for t in qTf]
        kT = [t[0:64, :] for t in kTf]
        vx = [A.bf16(16 * 128).rearrange("p (t c) -> p t c", t=16) for _ in range(2)]
        EB = [A.bf16(EBW) for _ in range(2)]
        den = [A.f32(8) for _ in range(2)]
        for b in range(2):
            self.MEMSET("vector", qTf[b], 0.0, ["qT%d" % b])
            self.MEMSET("vector", kTf[b], 0.0, ["kT%d" % b])
            self.MEMSET("vector", vx[b], 0.0, ["vx%d" % b])
            self.MEMSET("vector", vx[b][:, :, 0:65], 1.0, ["vx%d" % b])
        kvc = 0
        for seq in range(self.nseq):
            tok0 = seq * S
            for h in range(NH):
                hb = (seq * NH + h) % 2
                if h % 8 == 0:
                    kvb = kvc % 2
                    kvc += 1
                    kvh = h // 8
                    self.DMA("sync", kT[kvb], FT.ap()[1024 + kvh * 64:1024 + (kvh + 1) * 64, tok0:tok0 + S], [], ["kT%d" % kvb])
                    self.DMA("sync", vx[kvb][:, :, 0:64],
                             TM.ap()[tok0:tok0 + S, kvh * 64:(kvh + 1) * 64].rearrange("(t p) c -> p t c", p=128),
                             [], ["vx%d" % kvb])
                self.DMA("sync", qT[hb], FT.ap()[h * 64:(h + 1) * 64, tok0:tok0 + S], [], ["qT%d" % hb])
                self.DMA("sync", EB[hb], self.eb_ap(0, h), [], ["EB%d" % hb])

                def fin(m, acc, kacc, h=h):
                    d = den[self.acc_cnt % 2]
                    kd = "den%d" % (self.acc_cnt % 2)
                    self.TS("vector", d[:, 0:4], acc[:, :, 64], esink[:, h:h + 1], None, ALU.add, None, [kacc, "esink"], [kd])
                    self.RECIP(d[:, 4:8], d[:, 0:4], [kd], [kd])
                    self.TT("vector", O_sb[:, 4 * m:4 * m + 4, h * 64:(h + 1) * 64], acc[:, :, 0:64],
                            d[:, 4:8].unsqueeze(2).broadcast_to([128, 4, 64]), ALU.mult, [kacc, kd], ["O"])

                if getattr(self, "dbg", "") == "LOADS":
                    continue
                self.attend(qTf[hb], "qT%d" % hb, kTf[kvb], "kT%d" % kvb, vx[kvb], "vx%d" % kvb,
                            EB[hb], "EB%d" % hb, 128, fin)
            if getattr(self, "dbg", "") in ("LOADS", "ATT"):
                continue
            self.outproj(seq, O_sb, "O", wo, g_out, src, dst, bufs)


    def mixer_C(self, layer, j, src, dst, FT, TM):
        A = self.A
        w_in = self.din("c_w_in", [1, D, 3072]).ap()[j]
        w_out = self.din("c_w_out", [1, D, D]).ap()[j]
        mobac = self.din("mobac", [128, 3, 128])
        fm = [(g * 128, g * 128) for g in range(16)]
        tmc = [(2048, 512, 0), (2560, 512, 512)]
        self.proj(src, layer, w_in, 3072, fm, tmc, FT, TM)
        self.em.barrier()
        A.off = self.base_off
        g_out, wo, O_sb, bufs = self.attn_common(layer, w_out)
        cst = A.f32(3 * 128).rearrange("p (a c) -> p a c", a=3)
        self.DMA("sync", cst, mobac.ap(), [], ["cst"])
        PASTc = cst[:, 0, :].rearrange("p (t n) -> p t n", n=8)
        NEGBc = cst[:, 1, :].rearrange("p (t n) -> p t n", n=8)
        OWNc = cst[:, 2, :].rearrange("p (t n) -> p t n", n=8)
        FULL = 1
        qTf = [A.bf16(S) for _ in range(2)]
        kTf = [A.bf16(S) for _ in range(2)]
        for b in range(2):
            self.MEMSET("vector", qTf[b], 0.0, ["qT%d" % b])
            self.MEMSET("vector", kTf[b], 0.0, ["kT%d" % b])
        qT = [t[0:64, :] for t in qTf]
        kT = [t[0:64, :] for t in kTf]
        VW = 128 if FULL else 65
        vx = [A.bf16(16 * VW).rearrange("p (t c) -> p t c", t=16) for _ in range(2)]
        EB = [A.bf16(EBW) for _ in range(2)]
        den = [A.f32(8) for _ in range(2)]
        km32 = A.f32(8)[0:64, :]
        kmhi = A.bf16(8)[0:64, :]
        kmlo = A.bf16(8)[0:64, :]
        kmr = A.f32(8)[0:64, :]
        gm = A.f32(128).rearrange("p (t n) -> p t n", n=8)
        top8 = A.f32(128).rearrange("p (t n) -> p t n", n=8)
        sel = A.f32(128).rearrange("p (t n) -> p t n", n=8)
        mneg = A.bf16(128).rearrange("p (t n) -> p t n", n=8)
        MnegTf = [A.bf16(S) for _ in range(2)]
        MnegT = [t[0:8, :] for t in MnegTf]
        for b in range(2):
            self.MEMSET("vector", MnegTf[b], 0.0, ["MnegT%d" % b])
            if FULL:
                self.MEMSET("vector", vx[b], 0.0, ["vx%d" % b])
            self.MEMSET("vector", vx[b][:, :, 0:65], 1.0, ["vx%d" % b])
        ident = self.ident
        pg = self.pb(5)[:, 0:128].rearrange("p (t n) -> p t n", n=8)
        for seq in range(self.nseq):
            tok0 = seq * S
            for h in range(NH):
                hb = (seq * NH + h) % 2
                self.DMA("sync", kT[hb], FT.ap()[1024 + h * 64:1024 + (h + 1) * 64, tok0:tok0 + S], [], ["kT%d" % hb])
                self.DMA("sync", vx[hb][:, :, 0:64],
                         TM.ap()[tok0:tok0 + S, h * 64:(h + 1) * 64].rearrange("(t p) c -> p t c", p=128),
                         [], ["vx%d" % hb])
                self.DMA("sync", qT[hb], FT.ap()[h * 64:(h + 1) * 64, tok0:tok0 + S], [], ["qT%d" % hb])
                self.DMA("sync", EB[hb], self.eb_ap(1, h), [], ["EB%d" % hb])
                self.em.op("vector", lambda e, o=km32, i=kT[hb].rearrange("p (n k) -> p n k", n=8):
                           e.tensor_reduce(out=o, in_=i, axis=AX.X, op=ALU.add), ["kT%d" % hb], ["km"])
                self.TS("vector", km32, km32, 1.0 / 256, None, ALU.mult, None, ["km"], ["km"])
                self.CP("vector", kmhi, km32, ["km"], ["kmhi"])
                self.TT("vector", kmr, km32, kmhi, ALU.subtract, ["km", "kmhi"], ["kmr"])
                self.CP("vector", kmlo, kmr, ["kmr"], ["kmlo"])
                for qt in range(16):
                    self.MM(pg[:, qt, :], qT[hb][:, qt * 128:(qt + 1) * 128], kmhi, True, False,
                            ["qT%d" % hb, "kmhi"], [("ps", 5)])
                    self.MM(pg[:, qt, :], qT[hb][:, qt * 128:(qt + 1) * 128], kmlo, False, True,
                            ["qT%d" % hb, "kmlo"], [("ps", 5)])
                self.TT("vector", gm, pg, PASTc, ALU.mult, [("ps", 5), "cst"], ["gm"])
                self.TT("vector", gm, gm, NEGBc, ALU.add, ["gm", "cst"], ["gm"])
                for qt in range(16):
                    self.em.op("vector", lambda e, o=top8[:, qt, :], i=gm[:, qt, :]: e.max(out=o, in_=i), ["gm"], ["top8"])
                self.TT("vector", sel, gm, top8[:, :, 2:3].broadcast_to([128, 16, 8]), ALU.is_ge, ["gm", "top8"], ["sel"])
                self.TT("vector", sel, sel, OWNc, ALU.max, ["sel", "cst"], ["sel"])
                self.TS("vector", mneg, sel, 30000.0, -30000.0, ALU.mult, ALU.add, ["sel"], ["mneg"])
                pmt = self.pb(6).bitcast(BF16)
                for half in range(2):
                    for i in range(8):
                        qt = half * 8 + i
                        self.TR(pmt[0:8, i * 128:(i + 1) * 128], mneg[:, qt, :], ["mneg"], [("ps", 6)])
                    self.CP("scalar", MnegT[hb][:, half * 1024:(half + 1) * 1024], pmt[0:8, :], [("ps", 6)], ["MnegT%d" % hb])

                def fin(m, acc, kacc, h=h):
                    d = den[self.acc_cnt % 2]
                    kd = "den%d" % (self.acc_cnt % 2)
                    self.RECIP(d[:, 4:8], acc[:, :, 64], [kacc], [kd])
                    self.TT("vector", O_sb[:, 4 * m:4 * m + 4, h * 64:(h + 1) * 64], acc[:, :, 0:64],
                            d[:, 4:8].unsqueeze(2).broadcast_to([128, 4, 64]), ALU.mult, [kacc, kd], ["O"])

                def rowfn(kt):
                    n = kt // 2
                    if FULL:
                        return ident[:, n:n + 1].broadcast_to([128, 128])
                    return ident[0:8, n:n + 1].broadcast_to([8, 128])

                if FULL:
                    self.attend(qTf[hb], "qT%d" % hb, kTf[hb], "kT%d" % hb, vx[hb], "vx%d" % hb,
                                EB[hb], "EB%d" % hb, None, fin, mask=(MnegTf[hb], "MnegT%d" % hb, rowfn))
                else:
                    self.attend(qT[hb], "qT%d" % hb, kT[hb], "kT%d" % hb, vx[hb], "vx%d" % hb,
                                EB[hb], "EB%d" % hb, None, fin, mask=(MnegT[hb], "MnegT%d" % hb, rowfn))
            self.outproj(seq, O_sb, "O", wo, g_out, src, dst, bufs)


    def mixer_B(self, layer, j, src, dst, FT, TM):
        A = self.A
        em = self.em
        ident = self.ident
        w_in = self.din("b_w_in", [1, D, 2608]).ap()[j]
        w_out = self.din("b_w_out", [1, D, D]).ap()[j]
        cpos_d = self.din("b_cmp_pos", [1, 2, 32, 64]).ap()[j]
        w1_d = self.din("b_cmp_w1", [1, 2, 2048, 256]).ap()[j]
        w2_d = self.din("b_cmp_w2", [1, 2, 256, 64]).ap()[j]
        cmask_d = self.din("cmask", [127, S])
        ovl_d = self.din("ovl", [127, 32])
        selc_d = self.din("selc", [128, 2, 512])
        fm = [(g * 128, g * 128) for g in range(8)]
        fm += [(1024, 1024), (1152, 1152), (1280, 1280), (1408, 1408), (1536, 1536), (1664, 1664),
               (2048, 1792), (2176, 1920)]
        tmc = [(1792, 256, 0), (2304, 304, 256)]
        self.proj(src, layer, w_in, 2608, fm, tmc, FT, TM)
        self.em.barrier()
        A.off = self.base_off
        kcbTf = [A.bf16(4 * 128).rearrange("p (g n) -> p g n", g=4) for _ in range(self.nseq)]
        Rvf = [A.bf16(4 * 128).rearrange("p (g c) -> p g c", g=4) for _ in range(self.nseq)]
        kcbT = [t[0:64, :, 0:127] for t in kcbTf]
        Rv = [t[0:127, :, 0:97] for t in Rvf]
        keep_off = A.off
        ovl_sb = A.f32(32)[0:127, :]
        self.DMA("sync", ovl_sb, ovl_d.ap(), [], ["ovl"])
        for sq in range(self.nseq):
            self.MEMSET("vector", kcbTf[sq], 0.0, ["kcbT%d" % sq])
            self.MEMSET("vector", Rvf[sq], 0.0, ["Rv%d" % sq])
            self.MEMSET("vector", Rv[sq], 1.0, ["Rv%d" % sq])
            for g in range(4):
                self.CP("vector", Rv[sq][:, g, 65:97], ovl_sb, ["ovl", "Rv%d" % sq], ["Rv%d" % sq])
        w1 = A.bf16(32 * 256)[0:64, :].rearrange("p (l j) -> p l j", l=32)
        w2 = A.bf16(2 * 64).rearrange("p (h d) -> p h d", h=2)
        posn = A.f32(64)[0:32, :]
        posb = A.bf16(64)[0:32, :]
        posT = A.bf16(32)[0:64, :]
        cb = A.f32(2)
        xT = A.bf16(4 * S)[0:64, :].rearrange("p (g t) -> p g t", g=4)
        xh = A.f32(508)
        x2 = A.f32(508)
        sgm = A.f32(508)
        hid = [A.bf16(508) for _ in range(2)]
        pstb = self.pb(7).bitcast(BF16)
        for kv in range(2):
            self.DMA("gpsimd", w1, w1_d[kv].rearrange("(l d) j -> d l j", d=64), [], ["w1"])
            self.DMA("gpsimd", w2, w2_d[kv].rearrange("(h p) d -> p h d", p=128), [], ["w2"])
            self.DMA("sync", posn, cpos_d[kv], [], ["posn"])
            self.CP("vector", posb, posn, ["posn"], ["posb"])
            self.TR(pstb[0:64, 0:32], posb, ["posb"], [("ps", 7)])
            self.CP("scalar", posT, pstb[0:64, 0:32], [("ps", 7)], ["posT"])
            for jh in range(2):
                for l in range(32):
                    self.MM(self.pb(6)[:, jh:jh + 1], w1[:, l, jh * 128:(jh + 1) * 128], posT[:, l:l + 1],
                            l == 0, l == 31, ["w1", "posT"], [("ps", 6)])
            self.CP("vector", cb, self.pb(6)[:, 0:2], [("ps", 6)], ["cb"])
            for sq in range(self.nseq):
                tok0 = sq * S
                r0 = 1024 if kv == 0 else 1280
                self.DMA("sync", xT, FT.ap()[r0:r0 + 256, tok0:tok0 + S].rearrange("(g d) t -> d g t", d=64), [], ["xT"])
                for jh in range(2):
                    bank = jh
                    ph = self.pb(bank)[:, 0:508].rearrange("p (g n) -> p g n", g=4)
                    for g in range(4):
                        for l in range(32):
                            self.MM(ph[:, g, :], w1[:, l, jh * 128:(jh + 1) * 128], xT[:, g, l:l + 16 * 126 + 1:16],
                                    l == 0, l == 31, ["w1", "xT"], [("ps", bank)])
                    phf = self.pb(bank)[:, 0:508]
                    self.TS("vector", xh, phf, cb[:, jh:jh + 1], None, ALU.add, None, [("ps", bank), "cb"], ["xh"])
                    self.TT("vector", x2, xh, xh, ALU.mult, ["xh"], ["x2"])
                    self.TS("vector", x2, x2, 0.044715, 1.0, ALU.mult, ALU.add, ["x2"], ["x2"])
                    self.TT("vector", x2, x2, xh, ALU.mult, ["x2", "xh"], ["x2"])
                    self.ACT(sgm, x2, AF.Sigmoid, ["x2"], ["sgm"], scale=1.5957691216057308)
                    self.TT("vector", hid[jh], sgm, xh, ALU.mult, ["sgm", "xh"], ["hid%d" % jh])
                if kv == 0:
                    po = self.pb(2)[0:64, 0:508]
                    for jh in range(2):
                        self.MM(po, w2[:, jh, :], hid[jh], jh == 0, jh == 1, ["w2", "hid%d" % jh], [("ps", 2)])
                    self.CP("vector", kcbT[sq], po.rearrange("p (g n) -> p g n", g=4), [("ps", 2)], ["kcbT%d" % sq])
                else:
                    po = self.pb(2)[0:127, 0:256].rearrange("p (g d) -> p g d", g=4)
                    for g in range(4):
                        for jh in range(2):
                            self.MM(po[:, g, :], hid[jh][:, g * 127:(g + 1) * 127], w2[:, jh, :], jh == 0, jh == 1,
                                    ["w2", "hid%d" % jh], [("ps", 2)])
                    self.CP("vector", Rv[sq][:, :, 0:64], po, [("ps", 2)], ["Rv%d" % sq])
        self.em.barrier()
        A.off = keep_off
        g_out, wo, O_sb, bufs = self.attn_common(layer, w_out)
        CMf = A.bf16(S)
        CM = CMf[0:127, :]
        self.MEMSET("vector", CMf, 0.0, ["CM"])
        self.DMA("gpsimd", CM, cmask_d.ap(), [], ["CM"])
        selc = A.f32(1024).rearrange("p (a t j) -> p a t j", a=2, t=16)
        self.DMA("sync", selc.rearrange("p a t j -> p a (t j)"), selc_d.ap(), [], ["selc"])
        qTgf = [A.bf16(S) for _ in range(4)]
        ksTf = A.bf16(S)
        kwTf = A.bf16(S)
        qTg = [t[0:64, :] for t in qTgf]
        ksT = ksTf[0:64, :]
        kwT = kwTf[0:64, :]
        vs = A.bf16(16 * 128).rearrange("p (t c) -> p t c", t=16)
        vw = A.bf16(16 * 128).rearrange("p (t c) -> p t c", t=16)
        for r in range(4):
            self.MEMSET("vector", qTgf[r], 0.0, ["qT%d" % r])
        self.MEMSET("vector", ksTf, 0.0, ["ksT"])
        self.MEMSET("vector", kwTf, 0.0, ["kwT"])
        self.MEMSET("vector", vs, 0.0, ["vs"])
        self.MEMSET("vector", vw, 0.0, ["vw"])
        self.MEMSET("vector", vs[:, :, 0:65], 1.0, ["vs"])
        self.MEMSET("vector", vw[:, :, 0:65], 1.0, ["vw"])
        EB = [A.bf16(EBW) for _ in range(2)]
        graw = A.bf16(16 * 48).rearrange("p (t c) -> p t c", t=16)
        gsig = A.f32(16 * 48).rearrange("p (t c) -> p t c", t=16)
        imp = A.f32(16 * 32).rearrange("p (t j) -> p t j", t=16)
        impw = A.f32(16 * 32).rearrange("p (t j) -> p t j", t=16)
        impx = A.f32(16 * 32).rearrange("p (t j) -> p t j", t=16)
        m1 = A.f32(128).rearrange("p (t n) -> p t n", n=8)
        m2 = A.f32(128).rearrange("p (t n) -> p t n", n=8)
        mneg = A.bf16(16 * 32).rearrange("p (t j) -> p t j", t=16)
        MnegTf = A.bf16(S)
        MnegT = MnegTf[0:32, :]
        self.MEMSET("vector", MnegTf, 0.0, ["MnegT"])
        dd = [A.f32(16) for _ in range(2)]
        tb = [A.f32(256).rearrange("p (i d) -> p i d", i=4) for _ in range(2)]
        tcnt = [0]
        ebc = [0]

        Esel = A.bf16(16 * 128).rearrange("p (t k) -> p t k", t=16)
        for kt in range(16):
            self.CP("vector", Esel[:, kt, :].rearrange("p (a b) -> p a b", a=2),
                    ident[:, 2 * kt:2 * kt + 2].unsqueeze(2).broadcast_to([128, 2, 64]), ["ident"], ["Esel"])

        def rowfn(kt):
            return Esel[:, kt, :]

        for seq in range(self.nseq):
            tok0 = seq * S
            self.DMA("sync", graw, TM.ap()[tok0:tok0 + S, 512:560].rearrange("(t p) c -> p t c", p=128), [], ["graw"])
            self.ACT(gsig, graw, AF.Sigmoid, ["graw"], ["gsig"])
            for g in range(4):
                for r in range(4):
                    h = 4 * g + r
                    self.DMA("sync", qTg[r], FT.ap()[h * 64:(h + 1) * 64, tok0:tok0 + S], [], ["qT%d" % r])
                self.DMA("sync", ksT, FT.ap()[1536 + g * 64:1536 + (g + 1) * 64, tok0:tok0 + S], [], ["ksT"])
                self.DMA("sync", kwT, FT.ap()[1792 + g * 64:1792 + (g + 1) * 64, tok0:tok0 + S], [], ["kwT"])
                self.DMA("sync", vs[:, :, 0:64],
                         TM.ap()[tok0:tok0 + S, g * 64:(g + 1) * 64].rearrange("(t p) c -> p t c", p=128), [], ["vs"])
                self.DMA("sync", vw[:, :, 0:64],
                         TM.ap()[tok0:tok0 + S, 256 + g * 64:256 + (g + 1) * 64].rearrange("(t p) c -> p t c", p=128),
                         [], ["vw"])
                citems = [dict(r=r, m=m) for r in range(4) for m in range(4)]

                def cstage1(it):
                    r, m = it["r"], it["m"]
                    q0 = 512 * m
                    sb = self.st_cnt % 3
                    self.st_cnt += 1
                    it["sb"] = sb
                    ps = self.pb(sb)
                    kps = ("ps", sb)
                    self.MM(ps, kcbTf[seq][:, g, :], qTgf[r][:, q0:q0 + 512], True, True,
                            ["kcbT%d" % seq, "qT%d" % r], [kps])
                    self.ACT(self.E[sb], ps, AF.Exp, [kps], ["E%d" % sb], scale=0.125)
                    self.TT("vector", self.PT[sb], self.E[sb], CMf[:, q0:q0 + 512], ALU.mult,
                            ["E%d" % sb, "CM"], ["PT%d" % sb])

                def cstage2(it):
                    r, m, sb = it["r"], it["m"], it["sb"]
                    h = 4 * g + r
                    cbk = 5 + tcnt[0] % 2
                    kcb = ("ps", cbk)
                    accC = self.pb(cbk).rearrange("p (i c) -> p i c", i=4)
                    for i in range(4):
                        self.MM(accC[:, i, :], self.PT[sb][:, i * 128:(i + 1) * 128], Rvf[seq][:, g, :],
                                True, True, ["PT%d" % sb, "Rv%d" % seq], [kcb])
                    d = dd[tcnt[0] % 2]
                    kd = "dd%d" % (tcnt[0] % 2)
                    tcnt[0] += 1
                    self.TS("vector", d[:, 0:4], accC[:, :, 64], 1e-30, None, ALU.max, None, [kcb], [kd])
                    self.RECIP(d[:, 4:8], d[:, 0:4], [kd], [kd])
                    self.TT("vector", d[:, 8:12], d[:, 4:8], gsig[:, 4 * m:4 * m + 4, 3 * h], ALU.mult, [kd, "gsig"], [kd])
                    self.TT("vector", O_sb[:, 4 * m:4 * m + 4, h * 64:(h + 1) * 64], accC[:, :, 0:64],
                            d[:, 8:12].unsqueeze(2).broadcast_to([128, 4, 64]), ALU.mult, [kcb, kd], ["O"])
                    rb = d[:, 4:8].unsqueeze(2).broadcast_to([128, 4, 32])
                    if r == 0:
                        self.TT("vector", imp[:, 4 * m:4 * m + 4, :], accC[:, :, 65:97], rb, ALU.mult,
                                [kcb, kd], ["imp"])
                    else:
                        self.TT("vector", impx[:, 4 * m:4 * m + 4, :], accC[:, :, 65:97], rb, ALU.mult,
                                [kcb, kd], ["impx"])
                        self.TT("vector", imp[:, 4 * m:4 * m + 4, :], imp[:, 4 * m:4 * m + 4, :],
                                impx[:, 4 * m:4 * m + 4, :], ALU.add, ["imp", "impx"], ["imp"])

                for ci in range(len(citems) + 2):
                    if ci < len(citems):
                        cstage1(citems[ci])
                    if ci - 2 >= 0:
                        cstage2(citems[ci - 2])
                self.TT("vector", impw, imp, selc[:, 0], ALU.mult, ["imp", "selc"], ["impw"])
                self.TT("vector", impw, impw, selc[:, 1], ALU.add, ["impw", "selc"], ["impw"])
                for qt in range(16):
                    em.op("vector", lambda e, o=m1[:, qt, :], i=impw[:, qt, :]: e.max(out=o, in_=i), ["impw"], ["m1"])
                    em.op("vector", lambda e, o=impx[:, qt, :], t=m1[:, qt, :], i=impw[:, qt, :]:
                          e.match_replace(out=o, in_to_replace=t, in_values=i, imm_value=-3e9), ["impw", "m1"], ["impx"])
                    em.op("vector", lambda e, o=m2[:, qt, :], i=impx[:, qt, :]: e.max(out=o, in_=i), ["impx"], ["m2"])
                self.TT("vector", impx, impw, m2[:, :, 7:8].broadcast_to([128, 16, 32]), ALU.is_ge, ["impw", "m2"], ["impx"])
                self.TS("vector", mneg, impx, 30000.0, -30000.0, ALU.mult, ALU.add, ["impx"], ["mneg"])
                pmt = self.pb(7).bitcast(BF16)
                for half in range(2):
                    for i in range(8):
                        qt = half * 8 + i
                        self.TR(pmt[0:32, i * 128:(i + 1) * 128], mneg[:, qt, :], ["mneg"], [("ps", 7)])
                    self.CP("scalar", MnegT[:, half * 1024:(half + 1) * 1024], pmt[0:32, :], [("ps", 7)], ["MnegT"])
                for br in (1, 2):
                    for r in range(4):
                        h = 4 * g + r
                        eb = ebc[0] % 2
                        ebc[0] += 1
                        self.DMA("sync", EB[eb], self.eb_ap(br, h), [], ["EB%d" % eb])

                        def fin(m, acc, kacc, h=h, br=br):
                            d = dd[tcnt[0] % 2]
                            kd = "dd%d" % (tcnt[0] % 2)
                            t_ = tb[tcnt[0] % 2]
                            kt_ = "tb%d" % (tcnt[0] % 2)
                            tcnt[0] += 1
                            self.TS("vector", d[:, 0:4], acc[:, :, 64], 1e-30, None, ALU.max, None, [kacc], [kd])
                            self.RECIP(d[:, 4:8], d[:, 0:4], [kd], [kd])
                            self.TT("vector", d[:, 8:12], d[:, 4:8], gsig[:, 4 * m:4 * m + 4, 3 * h + br], ALU.mult,
                                    [kd, "gsig"], [kd])
                            self.TT("vector", t_, acc[:, :, 0:64], d[:, 8:12].unsqueeze(2).broadcast_to([128, 4, 64]),
                                    ALU.mult, [kacc, kd], [kt_])
                            osl = O_sb[:, 4 * m:4 * m + 4, h * 64:(h + 1) * 64]
                            self.TT("vector", osl, osl, t_, ALU.add, ["O", kt_], ["O"])

                        if br == 1:
                            self.attend(qTgf[r], "qT%d" % r, ksTf, "ksT", vs, "vs", EB[eb], "EB%d" % eb, None, fin,
                                        mask=(MnegTf, "MnegT", rowfn))
                        else:
                            self.attend(qTgf[r], "qT%d" % r, kwTf, "kwT", vw, "vw", EB[eb], "EB%d" % eb, 512, fin)
            self.outproj(seq, O_sb, "O", wo, g_out, src, dst, bufs)

def bucket_np(dist):
    dist = np.maximum(dist, 0)
    dd = np.maximum(dist, 1).astype(np.float32)
    lb = 16 + (np.log(dd / np.float32(16)) / np.float32(math.log(64)) * np.float32(16)).astype(np.int32)
    return np.where(dist < 16, dist, np.minimum(lb, 31))


def host_consts():
    U = REL_U
    dist = np.arange(U) - 127
    bk = bucket_np(dist)
    oh = np.zeros((3, 33, U), np.float32)
    for ty, win in enumerate((128, None, 512)):
        valid = dist >= 0
        if win is not None:
            valid &= dist < win
        for u in range(U):
            if valid[u]:
                oh[ty, bk[u], u] = 1.0
            else:
                oh[ty, 32, u] = 1.0
    mc = np.zeros((128, 3, 16, 8), np.float32)
    for qt in range(16):
        for n in range(8):
            past = 1.0 if n < qt // 2 else 0.0
            mc[:, 0, qt, n] = past
            mc[:, 1, qt, n] = -1e9 * (1.0 - past)
            mc[:, 2, qt, n] = 1.0 if n == qt // 2 else 0.0
    n_cmp = 127
    q = np.arange(S)
    cmask = ((np.arange(n_cmp)[:, None] * 16 + 31) <= q[None, :]).astype(np.float32)
    c_start = np.arange(n_cmp)[:, None] * 16
    s_start = np.arange(32)[None, :] * 64
    ovl = ((c_start < s_start + 64) & (c_start + 32 > s_start)).astype(np.float32)
    selc = np.zeros((128, 2, 16, 32), np.float32)
    for qt in range(16):
        for p in range(128):
            cur = (qt * 128 + p) // 64
            for jb in range(32):
                if jb > cur:
                    selc[p, 0, qt, jb] = 0.0
                    selc[p, 1, qt, jb] = -1e9 - 1024.0 * jb
                elif jb == 0 or jb == cur or jb == cur - 1:
                    selc[p, 0, qt, jb] = 0.0
                    selc[p, 1, qt, jb] = 1e9 + 1024.0 * jb
                else:
                    selc[p, 0, qt, jb] = 1.0
    return {"oh": oh, "mobac": mc.reshape(128, 3, 128), "cmask": cmask, "ovl": ovl,
            "selc": selc.reshape(128, 2, 512)}


FULL_PLAN = [("rel",)]
for _l in range(4):
    FULL_PLAN += [("ffn", _l, 0), ("mix", _l), ("ffn", _l, 1)]


def kernel(**inputs):
    n = 8
    nseq = 2
    b = Builder(nseq, FULL_PLAN)
    nc = b.build()
    x = np.ascontiguousarray(inputs["x"], dtype=np.float32)
    full = dict(inputs)
    full.update(host_consts())
    shared = {name: np.ascontiguousarray(full[name], dtype=np.float32) for name in b.ext_inputs if name != "x"}
    in_maps = []
    for c in range(n):
        m = dict(shared)
        m["x"] = np.ascontiguousarray(x[c * nseq:(c + 1) * nseq].reshape(nseq * S, D))
        in_maps.append(m)
    res = run_bass_kernel_spmd(nc, in_maps, core_ids=list(range(n)))
    out = np.concatenate([r["y"].reshape(nseq, S, D) for r in res.results], axis=0)
    return out.astype(np.float32)
```

```python
import math
from contextlib import ExitStack
import numpy as np
import concourse.bass as bass
import concourse.mybir as mybir
from concourse.bass_utils import run_bass_kernel_spmd

F32 = mybir.dt.float32
BF16 = mybir.dt.bfloat16
AF = mybir.ActivationFunctionType
ALU = mybir.AluOpType
AX = mybir.AxisListType

D = 1024
FF = 2816
S = 2048
NH = 16
DH = 64
EPS = 1e-6
MT = 1024
ARENA_WORDS = 50 * 1024
EBW = 1536
EBC = 1024
REL_U = EBW + 127

COMPUTE = ("tensor", "vector", "scalar", "gpsimd")
QUEUES = ("sync", "gpsimd", "scalar")
NPOOL = 12


class Em:
    def __init__(self, nc, es):
        self.nc = nc
        self.es = es
        self.ops = []

    def op(self, eng, fn, r=(), w=()):
        self.ops.append((eng, fn, tuple(r), tuple(w), 0))

    def dma(self, eng, fn, r=(), w=(), n=1):
        self.ops.append((eng, fn, tuple(r), tuple(w), n))

    def barrier(self):
        self.ops.append(("barrier", None, (), (), 0))

    def emit(self):
        nc, es = self.nc, self.es
        sems = []

        def newsem(name):
            sems.append(es.enter_context(nc.semaphore(name)))
            return len(sems) - 1

        esem = {e: newsem("c_" + e) for e in COMPUTE}
        pool = {q: [[newsem("d_%s%d" % (q, i)), 0] for i in range(NPOOL)] for q in QUEUES}
        pnext = {q: 0 for q in QUEUES}
        count = {e: 0 for e in COMPUTE}
        engs = ("tensor", "vector", "scalar", "gpsimd", "sync")
        known = {e: {} for e in engs}
        last_w = {}
        readers = {}
        streams = {e: [] for e in engs}

        def all_tokens():
            toks = []
            for q in QUEUES:
                for s, v in pool[q]:
                    if v > 0:
                        toks.append((s, v))
            for e in COMPUTE:
                if count[e] > 0:
                    toks.append((esem[e], count[e]))
            return toks

        for (eng, fn, r, w, ndma) in self.ops:
            if eng == "barrier":
                toks = all_tokens()
                for e in engs:
                    for s, v in toks:
                        if e == "tensor" and s == esem["tensor"]:
                            continue
                        if known[e].get(s, 0) >= v:
                            continue
                        streams[e].append(("wait", s, v))
                        known[e][s] = v
                last_w = {}
                readers = {}
                continue
            need = {}

            def add(tok):
                s, v = tok
                if need.get(s, 0) < v:
                    need[s] = v

            for k in r:
                if k in last_w:
                    add(last_w[k])
            for k in w:
                if k in last_w:
                    add(last_w[k])
                for s, v in readers.get(k, {}).items():
                    add((s, v))
            slot = None
            if ndma:
                slot = pool[eng][pnext[eng]]
                pnext[eng] = (pnext[eng] + 1) % NPOOL
                if slot[1] > 0:
                    add((slot[0], slot[1]))
            for s, v in need.items():
                if eng == "tensor" and s == esem["tensor"]:
                    continue
                if known[eng].get(s, 0) >= v:
                    continue
                streams[eng].append(("wait", s, v))
                known[eng][s] = v
            if ndma:
                slot[1] += 16 * ndma
                tok = (slot[0], slot[1])
            else:
                count[eng] += 1
                tok = (esem[eng], count[eng])
            streams[eng].append(("op", fn, tok, ndma))
            for k in w:
                last_w[k] = tok
                readers[k] = {}
            for k in r:
                d = readers.setdefault(k, {})
                if d.get(tok[0], 0) < tok[1]:
                    d[tok[0]] = tok[1]
        for s, v in all_tokens():
            if known["sync"].get(s, 0) < v:
                streams["sync"].append(("wait", s, v))
        self.n_instr = {e: len(streams[e]) for e in engs}
        self.streams = streams
        self.esem = esem

        def run(engobj, lst):
            for it in lst:
                if it[0] == "wait":
                    engobj.wait_ge(sems[it[1]], it[2])
                else:
                    _, fn, tok, ndma = it
                    res = fn(engobj)
                    if ndma:
                        assert len(res) == ndma, (len(res), ndma)
                        for ins in res:
                            ins.then_inc(sems[tok[0]], 16)
                    else:
                        res.then_inc(sems[tok[0]], 1)

        with nc.Block() as block:
            @block.tensor
            def _(e):
                run(e, streams["tensor"])

            @block.vector
            def _(e):
                run(e, streams["vector"])

            @block.scalar
            def _(e):
                run(e, streams["scalar"])

            @block.gpsimd
            def _(e):
                run(e, streams["gpsimd"])

            @block.sync
            def _(e):
                run(e, streams["sync"])


class Arena:
    def __init__(self, t):
        self.t = t
        self.off = 0

    def f32(self, n):
        v = self.t[:, self.off:self.off + n]
        self.off += n
        assert self.off <= ARENA_WORDS, self.off
        return v

    def bf16(self, n):
        words = (n + 1) // 2
        v = self.t[:, self.off:self.off + words].bitcast(BF16)
        self.off += words
        assert self.off <= ARENA_WORDS, self.off
        return v[:, 0:n]


class Builder:
    def __init__(self, nseq, plan):
        self.nseq = nseq
        self.ntok = nseq * S
        self.plan = plan
        self.nc = bass.Bass("TRN2", target_bir_lowering=False)
        self.dram = {}
        self.ext_inputs = []

    def din(self, name, shape, dt=F32):
        if name not in self.dram:
            self.dram[name] = self.nc.dram_tensor(name, list(shape), dt, kind="ExternalInput")
            self.ext_inputs.append(name)
        return self.dram[name]

    def dscratch(self, name, shape, dt=F32):
        if name not in self.dram:
            self.dram[name] = self.nc.dram_tensor(name, list(shape), dt, kind="Internal")
        return self.dram[name]

    def MM(self, out, lhsT, rhs, start, stop, r, w, skip=False):
        if skip:
            self.em.op("tensor", lambda e: e.matmul(out, lhsT=lhsT, rhs=rhs, start=start, stop=stop,
                                                    skip_group_check=True), r, w)
        else:
            self.em.op("tensor", lambda e: e.matmul(out, lhsT=lhsT, rhs=rhs, start=start, stop=stop), r, w)

    def TR(self, out, in_, r, w):
        k = in_.shape[0]
        ident = self.ident[0:k, 0:k]
        self.em.op("tensor", lambda e: e.transpose(out=out, in_=in_, identity=ident), tuple(r) + ("ident",), w)

    def TRF(self, out, in_, r, w):
        k = in_.shape[0]
        ident = self.identf[0:k, 0:k]
        self.em.op("tensor", lambda e: e.transpose(out=out, in_=in_, identity=ident), tuple(r) + ("identf",), w)

    def ACT(self, out, in_, func, r, w, **kw):
        self.em.op("scalar", lambda e: e.activation(out=out, in_=in_, func=func, **kw), r, w)

    def TT(self, eng, out, in0, in1, op, r, w):
        self.em.op(eng, lambda e: e.tensor_tensor(out=out, in0=in0, in1=in1, op=op), r, w)

    def TS(self, eng, out, in0, s1, s2, op0, op1, r, w):
        if s2 is None:
            self.em.op(eng, lambda e: e.tensor_scalar(out=out, in0=in0, scalar1=s1, scalar2=None, op0=op0), r, w)
        else:
            self.em.op(eng, lambda e: e.tensor_scalar(out=out, in0=in0, scalar1=s1, scalar2=s2, op0=op0, op1=op1), r, w)

    def STT(self, eng, out, in0, scalar, in1, op0, op1, r, w):
        self.em.op(eng, lambda e: e.scalar_tensor_tensor(out=out, in0=in0, scalar=scalar, in1=in1, op0=op0, op1=op1), r, w)

    def CP(self, eng, out, in_, r, w):
        if eng == "scalar":
            self.em.op(eng, lambda e: e.copy(out=out, in_=in_), r, w)
        else:
            self.em.op(eng, lambda e: e.tensor_copy(out=out, in_=in_), r, w)

    def RECIP(self, out, in_, r, w):
        self.em.op("vector", lambda e: e.reciprocal(out=out, in_=in_), r, w)

    def MEMSET(self, eng, ap, val, w):
        self.em.op(eng, lambda e: e.memset(ap, val), (), w)

    def DMA(self, eng, out, in_, r, w):
        self.em.dma(eng, lambda e: [e.dma_start(out=out, in_=in_)], r, w, n=1)

    def RSTD(self, ss, tmp, rs, key):
        self.TS("vector", tmp, ss, 1.0 / D, EPS, ALU.mult, ALU.add, [key], [key])
        self.ACT(tmp, tmp, AF.Sqrt, [key], [key])
        self.RECIP(rs, tmp, [key], [key])

    def build(self):
        nc = self.nc
        with ExitStack() as es:
            self.es = es
            self.em = Em(nc, es)
            arena_t = es.enter_context(nc.sbuf_tensor("arena", [128, ARENA_WORDS], F32))
            self.psum = es.enter_context(nc.psum_tensor("psum", [128, 4096], F32))
            self.A = Arena(arena_t)
            self.x_in = self.din("x", [self.ntok, D])
            self.y = self.nc.dram_tensor("y", [self.ntok, D], F32, kind="ExternalOutput")
            self.norm_g = self.din("norm_g", [4, 6, D])
            self.setup_consts()
            self.base_off = self.A.off
            cur = self.x_in
            for ph in self.plan:
                self.A.off = self.base_off
                self.em.barrier()
                if ph[0] == "ffn":
                    self.ffn(ph[1], ph[2], cur, self.y)
                    cur = self.y
                elif ph[0] == "copy":
                    self.copy_phase(cur, self.y)
                    cur = self.y
                elif ph[0] == "rel":
                    self.rel_phase()
                elif ph[0] == "mix":
                    self.mixer(ph[1], cur, self.y)
                    cur = self.y
                else:
                    raise ValueError(ph)
            self.em.emit()
        return nc

    def pb(self, i, n=1):
        return self.psum[:, 512 * i:512 * (i + n)]

    def setup_consts(self):
        A = self.A
        self.ident = A.bf16(128)
        identf = A.f32(128)
        self.identf = identf
        self.MEMSET("gpsimd", identf, 0.0, ["identf"])
        self.em.op("gpsimd", lambda e: e.affine_select(out=identf, in_=identf, pattern=[[-1, 128]],
                   compare_op=ALU.not_equal, fill=1.0, base=0, channel_multiplier=1), ["identf"], ["identf"])
        self.CP("vector", self.ident, identf, ["identf"], ["ident"])

    def copy_phase(self, src, dst):
        A = self.A
        bufs = [A.f32(D) for _ in range(2)]
        for t in range(self.ntok // 128):
            b = t % 2
            self.DMA("sync", bufs[b], src.ap()[t * 128:(t + 1) * 128, :], [], ["cb%d" % b])
            self.DMA("sync", dst.ap()[t * 128:(t + 1) * 128, :], bufs[b], ["cb%d" % b], [])

    def ffn(self, layer, which, src, dst):
        A = self.A
        em = self.em
        w_in = self.din("ffn_w_in", [4, 2, D, 2 * FF]).ap()[layer, which]
        w_out = self.din("ffn_w_out", [4, 2, FF, D]).ap()[layer, which]
        gi = 0 if which == 0 else 4
        g_in = A.f32(D)
        g_out = A.f32(D)
        self.DMA("sync", g_in, self.norm_g.ap()[layer, gi:gi + 1, :].broadcast_to([128, D]), [], ["g_in"])
        self.DMA("sync", g_out, self.norm_g.ap()[layer, gi + 1:gi + 2, :].broadcast_to([128, D]), [], ["g_out"])
        self.TS("vector", g_out, g_out, 0.5, None, ALU.mult, None, ["g_out"], ["g_out"])
        xa = [A.f32(D) for _ in range(2)]
        junk = A.f32(D)
        hn = [A.bf16(D) for _ in range(2)]
        hT = A.bf16(8 * MT).rearrange("p (k t) -> p k t", k=8)
        aT = A.bf16(22 * MT).rearrange("p (c t) -> p c t", c=22)
        wi = [A.bf16(8 * 1024).rearrange("p (k f) -> p k f", k=8) for _ in range(2)]
        wo = A.bf16(22 * D).rearrange("p (c d) -> p c d", c=22)
        sg = [A.f32(512) for _ in range(2)]
        st = A.f32(64)
        xe = [A.f32(D) for _ in range(2)]
        tmp = [A.f32(D) for _ in range(2)]
        xo = [A.f32(D) for _ in range(2)]
        w_in_v = w_in.rearrange("(k p) f -> p k f", p=128)
        w_out_v = w_out.rearrange("(c p) d -> p c d", p=128)
        nmt = self.ntok // MT
        ngrp = 6
        grp_chunks = [list(range(g * 4, min(22, g * 4 + 4))) for g in range(ngrp)]
        stream = [(m, g) for m in range(nmt) for g in range(ngrp)]

        def load_group(idx):
            m, g = stream[idx]
            b = idx % 2
            ch = grp_chunks[g]
            ncol = 128 * len(ch)
            c0 = ch[0] * 128
            em.dma("gpsimd", lambda e: [
                e.dma_start(out=wi[b][:, :, 0:ncol], in_=w_in_v[:, :, c0:c0 + ncol]),
                e.dma_start(out=wi[b][:, :, 512:512 + ncol], in_=w_in_v[:, :, FF + c0:FF + c0 + ncol]),
            ], [], [("wi", b)], n=2)

        pst = self.pb(7).bitcast(BF16)
        load_group(0)
        for m in range(nmt):
            for s in range(8):
                t0 = m * MT + s * 128
                b = s % 2
                kx = "xa%d" % b
                ks = "st%d" % s
                if m == 0 and s == 0:
                    self.DMA("sync", xa[0], src.ap()[0:128, :], [], ["xa0"])
                if t0 + 128 < self.ntok:
                    self.DMA("sync", xa[(s + 1) % 2], src.ap()[t0 + 128:t0 + 256, :], [], ["xa%d" % ((s + 1) % 2)])
                self.MEMSET("vector", st[:, s:s + 1], 0.0, [ks])
                self.ACT(junk, xa[b], AF.Square, [kx, ks], ["junk", ks], accum_out=st[:, s:s + 1])
                self.RSTD(st[:, s:s + 1], st[:, 8 + s:9 + s], st[:, 16 + s:17 + s], ks)
                self.STT("vector", hn[b], xa[b], st[:, 16 + s:17 + s], g_in, ALU.mult, ALU.mult,
                         [kx, ks, "g_in"], ["hn%d" % b])
                for kc in range(8):
                    self.TR(pst[:, kc * 128:(kc + 1) * 128], hn[b][:, kc * 128:(kc + 1) * 128],
                            ["hn%d" % b], [("ps", 7)])
                self.CP("scalar", hT[:, :, s * 128:(s + 1) * 128],
                        pst.rearrange("p (k t) -> p k t", k=8), [("ps", 7)], [("hT", s)])
            if getattr(self, "dbg", "") == "B":
                continue
            for g in range(ngrp):
                idx = m * ngrp + g
                if idx + 1 < len(stream):
                    load_group(idx + 1)
                b = idx % 2
                for ci, c in enumerate(grp_chunks[g]):
                    self.DMA("gpsimd", wo[:, c, :], w_out_v[:, c, :], [], [("wo", c)])
                    for tt in range(2):
                        pg = self.pb(2 * tt)
                        pu = self.pb(2 * tt + 1)
                        hkeys = [("hT", 4 * tt + i) for i in range(4)]
                        for kc in range(8):
                            self.MM(pg, wi[b][:, kc, ci * 128:(ci + 1) * 128], hT[:, kc, tt * 512:(tt + 1) * 512],
                                    kc == 0, kc == 7, [("wi", b)] + hkeys, [("ps", 2 * tt)])
                        for kc in range(8):
                            self.MM(pu, wi[b][:, kc, 512 + ci * 128:512 + (ci + 1) * 128],
                                    hT[:, kc, tt * 512:(tt + 1) * 512],
                                    kc == 0, kc == 7, [("wi", b)] + hkeys, [("ps", 2 * tt + 1)])
                        self.ACT(sg[tt], pg, AF.Silu, [("ps", 2 * tt)], ["sg%d" % tt])
                        self.TT("vector", aT[:, c, tt * 512:(tt + 1) * 512], sg[tt], pu, ALU.mult,
                                ["sg%d" % tt, ("ps", 2 * tt + 1)], [("aT", c, tt)])
            if getattr(self, "dbg", "") == "BC":
                continue
            for s in range(8):
                t0 = m * MT + s * 128
                b = s % 2
                tt = s // 4
                pm = self.pb(4 + 2 * b, 2)
                kpm = ("ps", 4 + 2 * b)
                for nh in range(2):
                    for c in range(22):
                        self.MM(pm[:, nh * 512:(nh + 1) * 512], aT[:, c, s * 128:(s + 1) * 128],
                                wo[:, c, nh * 512:(nh + 1) * 512], c == 0, c == 21,
                                [("aT", c, tt), ("wo", c)], [kpm, ("ps", 5 + 2 * b)])
                ks = "st2%d" % s
                self.DMA("sync", xe[b], src.ap()[t0:t0 + 128, :], [], ["xe%d" % b])
                self.MEMSET("vector", st[:, 24 + s:25 + s], 0.0, [ks])
                for nh in range(2):
                    self.CP("vector", tmp[b][:, nh * 512:(nh + 1) * 512], pm[:, nh * 512:(nh + 1) * 512], [kpm], ["tmp%d" % b])
                self.ACT(junk, tmp[b], AF.Square, ["tmp%d" % b, ks], ["junk", ks], accum_out=st[:, 24 + s:25 + s])
                self.RSTD(st[:, 24 + s:25 + s], st[:, 32 + s:33 + s], st[:, 40 + s:41 + s], ks)
                self.STT("vector", xo[b], tmp[b], st[:, 40 + s:41 + s], g_out, ALU.mult, ALU.mult,
                         ["tmp%d" % b, ks, "g_out"], ["xo%d" % b])
                self.TT("vector", xo[b], xo[b], xe[b], ALU.add, ["xo%d" % b, "xe%d" % b], ["xo%d" % b])
                self.DMA("sync", dst.ap()[t0:t0 + 128, :], xo[b], ["xo%d" % b], [])


    def norm_load(self, src, t0, b, xa, queue="sync"):
        self.DMA(queue, xa[b], src.ap()[t0:t0 + 128, :], [], ["xa%d" % b])

    def norm_T(self, src, t0, s, g_in, xa, junk, hn, st, hT, pst, hkey=None, loaded=False, b=None):
        if b is None:
            b = s % 2
        kx = "xa%d" % b
        ks = "st%d" % s
        if not loaded:
            self.DMA("sync", xa[b], src.ap()[t0:t0 + 128, :], [], [kx])
        self.MEMSET("vector", st[:, s:s + 1], 0.0, [ks])
        self.ACT(junk, xa[b], AF.Square, [kx, ks], ["junk", ks], accum_out=st[:, s:s + 1])
        self.RSTD(st[:, s:s + 1], st[:, 8 + s:9 + s], st[:, 16 + s:17 + s], ks)
        self.STT("vector", hn[b], xa[b], st[:, 16 + s:17 + s], g_in, ALU.mult, ALU.mult,
                 [kx, ks, "g_in"], ["hn%d" % b])
        for kc in range(8):
            self.TR(pst[:, kc * 128:(kc + 1) * 128], hn[b][:, kc * 128:(kc + 1) * 128],
                    ["hn%d" % b], [("ps", 7)])
        self.CP("scalar", hT[:, :, s * 128:(s + 1) * 128],
                pst.rearrange("p (k t) -> p k t", k=8), [("ps", 7)], [hkey if hkey is not None else ("hT", s)])

    def rel_phase(self):
        A = self.A
        U = REL_U
        oh = self.din("oh", [3, 33, U])
        rel = self.din("rel_bias", [32, NH])
        Z = self.dscratch("Z", [3, NH, 128, U], BF16)
        tab = A.f32(NH)[0:33, :]
        self.MEMSET("vector", tab, -30000.0, ["tab"])
        self.DMA("sync", tab[0:32, :], rel.ap(), [], ["tab"])
        oht = A.f32(U)[0:33, :]
        Gs = A.bf16(U)[0:16, :]
        for ty in range(3):
            self.DMA("sync", oht, oh.ap()[ty], [], ["oht"])
            for c0 in range(0, U, 512):
                n = min(512, U - c0)
                bank = (c0 // 512) % 4
                self.MM(self.pb(bank)[0:16, 0:n], tab, oht[:, c0:c0 + n], True, True, ["tab", "oht"], [("ps", bank)])
                self.ACT(Gs[:, c0:c0 + n], self.pb(bank)[0:16, 0:n], AF.Exp, [("ps", bank)], ["Gs"])
            self.DMA("sync", Z.ap()[ty], Gs.unsqueeze(1).broadcast_to([NH, 128, U]), ["Gs"], [])

    def eb_ap(self, ty, h):
        Z = self.dram["Z"]
        U = REL_U
        return bass.AP(tensor=Z, offset=((ty * NH + h) * 128) * U + 127, ap=[[U - 1, 128], [1, EBW]])

    def proj(self, src, layer, w_ap, ncols, fm_groups, tm_chunks, FT, TM):
        A = self.A
        em = self.em
        g_in = A.f32(D)
        self.DMA("sync", g_in, self.norm_g.ap()[layer, 2:3, :].broadcast_to([128, D]), [], ["g_in"])
        w_sb = A.bf16(8 * ncols).rearrange("p (k f) -> p k f", k=8)
        w_v = w_ap.rearrange("(k p) f -> p k f", p=128)
        for c0 in range(0, ncols, 512):
            c1 = min(ncols, c0 + 512)
            self.DMA("gpsimd", w_sb[:, :, c0:c1], w_v[:, :, c0:c1], [], [("w", c0 // 512)])

        def wkeys(c, n):
            return [("w", i) for i in range(c // 512, (c + n - 1) // 512 + 1)]

        xa = [A.f32(D) for _ in range(2)]
        junk = A.f32(D)
        hn = [A.bf16(D) for _ in range(2)]
        hTs = [A.bf16(8 * 512).rearrange("p (k t) -> p k t", k=8) for _ in range(2)]
        st = A.f32(64)
        stg = [A.bf16(512) for _ in range(3)]
        stg2 = [A.bf16(512) for _ in range(3)]
        pst = self.pb(7).bitcast(BF16)
        cnt = [0]
        nmt = self.ntok // 512

        nidx = [0]

        def norm_item(mt, s_):
            hb = mt % 2
            k = nidx[0]
            nidx[0] += 1
            assert k == mt * 4 + s_
            if k == 0:
                self.norm_load(src, 0, 0, xa, "gpsimd")
            if k + 1 < nmt * 4:
                self.norm_load(src, (k + 1) * 128, (k + 1) % 2, xa, "gpsimd")
            self.norm_T(src, mt * 512 + s_ * 128, s_, g_in, xa, junk, hn, st, hTs[hb], pst, hkey=("hT", hb, s_),
                        loaded=True, b=k % 2)

        def fm_item(mt, gi, wc, frow):
            hb = mt % 2
            hT = hTs[hb]
            t0 = mt * 512
            hkeys = [("hT", hb, i) for i in range(4)]
            bank = gi % 3
            b3 = gi % 3
            for kc in range(8):
                self.MM(self.pb(bank), w_sb[:, kc, wc:wc + 128], hT[:, kc, :], kc == 0, kc == 7,
                        wkeys(wc, 128) + hkeys, [("ps", bank)])
            self.CP("scalar" if gi % 2 == 0 else "vector", stg[b3], self.pb(bank), [("ps", bank)], ["stg%d" % b3])
            self.DMA("sync", FT.ap()[frow:frow + 128, t0:t0 + 512], stg[b3], ["stg%d" % b3], [])

        def tm_item(mt, s_, wc, n, tcol):
            hb = mt % 2
            hT = hTs[hb]
            t0 = mt * 512
            bank = 3 + cnt[0] % 3
            b3 = cnt[0] % 3
            cnt[0] += 1
            for kc in range(8):
                self.MM(self.pb(bank)[:, 0:n], hT[:, kc, s_ * 128:(s_ + 1) * 128], w_sb[:, kc, wc:wc + n],
                        kc == 0, kc == 7, wkeys(wc, n) + [("hT", hb, s_)], [("ps", bank)])
            self.CP("vector" if cnt[0] % 2 == 0 else "scalar", stg2[b3][:, 0:n], self.pb(bank)[:, 0:n],
                    [("ps", bank)], ["stg2%d" % b3])
            self.DMA("sync", TM.ap()[t0 + s_ * 128:t0 + (s_ + 1) * 128, tcol:tcol + n], stg2[b3][:, 0:n],
                     ["stg2%d" % b3], [])

        for s_ in range(4):
            norm_item(0, s_)
        for mt in range(nmt):
            items = []
            for gi, (wc, frow) in enumerate(fm_groups):
                items.append(lambda mt=mt, gi=gi, wc=wc, frow=frow: fm_item(mt, gi, wc, frow))
            for s_ in range(4):
                for (wc, n, tcol) in tm_chunks:
                    items.append(lambda mt=mt, s_=s_, wc=wc, n=n, tcol=tcol: tm_item(mt, s_, wc, n, tcol))
            step = max(1, len(items) // 4)
            ns = 0
            for ii, it in enumerate(items):
                it()
                if mt + 1 < nmt and ns < 4 and (ii + 1) % step == 0:
                    norm_item(mt + 1, ns)
                    ns += 1
            while mt + 1 < nmt and ns < 4:
                norm_item(mt + 1, ns)
                ns += 1

    def attend(self, qT, kq, kT, kk, vext, kv, EB, keb, window, fin, mask=None):
        E = self.E
        PT = self.PT
        LA = 2
        flat = []
        rounds = {}
        for m in range(4):
            q0 = 512 * m
            tiles = []
            for kt in range(16):
                j0 = max(0, 128 * kt - q0)
                j1 = 512 if window is None else min(512, 128 * kt + 127 + window - q0)
                j1 = ((j1 + 127) // 128) * 128
                if j1 > j0:
                    tiles.append((kt, j0, j1))
            last = {}
            for (kt, j0, j1) in tiles:
                for i in range(j0 // 128, j1 // 128):
                    last[i] = kt
            ab = 3 + self.acc_cnt % 2
            self.acc_cnt += 1
            rounds[m] = dict(last=last, ab=ab, opened=False, n=len(tiles), done=0)
            for (kt, j0, j1) in tiles:
                flat.append(dict(m=m, kt=kt, j0=j0, j1=j1))

        def stage1(t):
            m, kt, j0, j1 = t["m"], t["kt"], t["j0"], t["j1"]
            q0 = 512 * m
            n = j1 - j0
            sb = self.st_cnt % 3
            self.st_cnt += 1
            t["sb"] = sb
            ps = self.pb(sb)[:, 0:n]
            kps = ("ps", sb)
            self.MM(ps, kT[:, kt * 128:(kt + 1) * 128], qT[:, q0 + j0:q0 + j1], True, mask is None, [kk, kq], [kps])
            if mask is not None:
                MnegT, kmask, rowfn = mask
                self.MM(ps, rowfn(kt), MnegT[:, q0 + j0:q0 + j1], False, True, [kmask, "ident", "Esel"], [kps])
            self.ACT(E[sb][:, 0:n], ps, AF.Exp, [kps], ["E%d" % sb], scale=0.125)
            c0 = min(q0 + j0 - 128 * kt, EBC)
            self.TT("vector", PT[sb][:, 0:n], E[sb][:, 0:n], EB[:, c0:c0 + n], ALU.mult, ["E%d" % sb, keb], ["PT%d" % sb])

        def stage2(t):
            m, kt, j0, j1, sb = t["m"], t["kt"], t["j0"], t["j1"], t["sb"]
            rd = rounds[m]
            n = j1 - j0
            oT = self.pb(rd["ab"])[0:vext.shape[2], :]
            kacc = ("ps", rd["ab"])
            self.MM(oT[:, j0:j1], vext[:, kt, :], PT[sb][:, 0:n], not rd["opened"], True, ["PT%d" % sb, kv], [kacc], skip=True)
            rd["opened"] = True
            rd["done"] += 1
            if rd["done"] == rd["n"]:
                ob = self.t_cnt % 2
                tbk = 5 + self.t_cnt % 2
                self.t_cnt += 1
                oTs = self.oTs[ob][0:65, :]
                self.CP("scalar", oTs, oT[0:65, :], [kacc], ["oTs%d" % ob])

                def finish(m=m, ob=ob, tbk=tbk, oTs=oTs):
                    T = self.pb(tbk).rearrange("p (i c) -> p i c", i=4)
                    for i in range(4):
                        self.TRF(T[:, i, 0:65], oTs[:, i * 128:(i + 1) * 128], ["oTs%d" % ob], [("ps", tbk)])
                    fin(m, T, ("ps", tbk))

                pending.append([2, finish])

        for _ in range(getattr(self, "warm", 0)):
            self.MM(self.pb(7), self.ident, self.wo_c[:, 0, 0:512], True, True, ["ident", "wo"], [("ps", 7)])
        pending = []
        for ti in range(len(flat) + LA):
            if ti < len(flat):
                stage1(flat[ti])
            for p_ in pending:
                p_[0] -= 1
            while pending and pending[0][0] <= 0:
                pending.pop(0)[1]()
            if ti - LA >= 0:
                stage2(flat[ti - LA])
        while pending:
            pending.pop(0)[1]()

    def outproj(self, seq, O_sb, kO, wo, g_out, src, dst, bufs):
        OT, junk, tmp, xe, xo, st = bufs
        pst = self.pb(7).bitcast(BF16)
        for s in range(16):
            if getattr(self, "dbg", "") == "OUT1" and s >= 1:
                break
            t0 = seq * S + s * 128
            b = s % 2
            for kc in range(8):
                self.TR(pst[:, kc * 128:(kc + 1) * 128], O_sb[:, s, kc * 128:(kc + 1) * 128], [kO], [("ps", 7)])
            self.CP("scalar", OT[b], pst, [("ps", 7)], ["OT%d" % b])
            pm = self.pb(5, 2) if False else None
            banks = (5, 6)
            for nh in range(2):
                for kc in range(8):
                    self.MM(self.pb(banks[nh]), OT[b][:, kc * 128:(kc + 1) * 128], wo[:, kc, nh * 512:(nh + 1) * 512],
                            kc == 0, kc == 7, ["OT%d" % b, "wo"], [("ps", banks[nh])])
            if getattr(self, "dbg2", "") == "a":
                continue
            ks = "ost%d" % b
            self.DMA("sync", xe[b], src.ap()[t0:t0 + 128, :], [], ["xe%d" % b])
            self.MEMSET("vector", st[:, 8 * b:8 * b + 1], 0.0, [ks])
            for nh in range(2):
                self.CP("vector", tmp[b][:, nh * 512:(nh + 1) * 512], self.pb(banks[nh]), [("ps", banks[nh])], ["tmp%d" % b])
            self.ACT(junk, tmp[b], AF.Square, ["tmp%d" % b, ks], ["junk", ks], accum_out=st[:, 8 * b:8 * b + 1])
            self.RSTD(st[:, 8 * b:8 * b + 1], st[:, 8 * b + 2:8 * b + 3], st[:, 8 * b + 3:8 * b + 4], ks)
            self.STT("vector", xo[b], tmp[b], st[:, 8 * b + 3:8 * b + 4], g_out, ALU.mult, ALU.mult,
                     ["tmp%d" % b, ks, "g_out"], ["xo%d" % b])
            self.TT("vector", xo[b], xo[b], xe[b], ALU.add, ["xo%d" % b, "xe%d" % b], ["xo%d" % b])
            self.DMA("sync", dst.ap()[t0:t0 + 128, :], xo[b], ["xo%d" % b], [])

    def attn_common(self, layer, w_out_ap):
        A = self.A
        g_out = A.f32(D)
        self.DMA("sync", g_out, self.norm_g.ap()[layer, 3:4, :].broadcast_to([128, D]), [], ["g_out"])
        wo = A.bf16(8 * D).rearrange("p (k d) -> p k d", k=8)
        self.wo_c = wo
        self.DMA("gpsimd", wo, w_out_ap.rearrange("(k p) d -> p k d", p=128), [], ["wo"])
        O_sb = A.bf16(16 * D).rearrange("p (s d) -> p s d", s=16)
        self.E = [A.bf16(512) for _ in range(3)]
        self.oTs = [A.f32(512) for _ in range(2)]
        self.t_cnt = 0
        self.PT = [A.bf16(512) for _ in range(3)]
        OT = [A.bf16(D) for _ in range(2)]
        junk = A.f32(D)
        tmp = [A.f32(D) for _ in range(2)]
        xe = [A.f32(D) for _ in range(2)]
        xo = [A.f32(D) for _ in range(2)]
        st = A.f32(16)
        self.acc_cnt = 0
        self.st_cnt = 0
        return g_out, wo, O_sb, (OT, junk, tmp, xe, xo, st)

    def mixer(self, layer, src, dst):
        kind, j = layer % 3, layer // 3
        FT = self.dscratch("FT", [2048, self.ntok], BF16)
        TM = self.dscratch("TM", [self.ntok, 1024], BF16)
        if kind == 0:
            self.mixer_A(layer, j, src, dst, FT, TM)
        elif kind == 2:
            self.mixer_C(layer, j, src, dst, FT, TM)
        else:
            self.mixer_B(layer, j, src, dst, FT, TM)

    def mixer_A(self, layer, j, src, dst, FT, TM):
        A = self.A
        w_in = self.din("a_w_in", [2, D, 1280]).ap()[j]
        w_out = self.din("a_w_out", [2, D, D]).ap()[j]
        sinks = self.din("a_sinks", [2, NH]).ap()
        fm = [(g * 128, g * 128) for g in range(9)]
        tmc = [(1152, 128, 0)]
        self.proj(src, layer, w_in, 1280, fm, tmc, FT, TM)
        if getattr(self, "dbg", "") == "P1":
            return
        self.em.barrier()
        A.off = self.base_off
        g_out, wo, O_sb, bufs = self.attn_common(layer, w_out)
        esink = A.f32(NH)
        self.DMA("sync", esink, sinks[j:j + 1, :].broadcast_to([128, NH]), [], ["esink"])
        self.ACT(esink, esink, AF.Exp, ["esink"], ["esink"])
        qTf = [A.bf16(S) for _ in range(2)]
        kTf = [A.bf16(S) for _ in range(2)]
        qT = [t[0:64, :] for t in qTf]
        kT = [t[0:64, :] for t in kTf]
        vx = [A.bf16(16 * 128).rearrange("p (t c) -> p t c", t=16) for _ in range(2)]
        EB = [A.bf16(EBW) for _ in range(2)]
        den = [A.f32(8) for _ in range(2)]
        for b in range(2):
            self.MEMSET("vector", qTf[b], 0.0, ["qT%d" % b])
            self.MEMSET("vector", kTf[b], 0.0, ["kT%d" % b])
            self.MEMSET("vector", vx[b], 0.0, ["vx%d" % b])
            self.MEMSET("vector", vx[b][:, :, 0:65], 1.0, ["vx%d" % b])
        kvc = 0
        for seq in range(self.nseq):
            tok0 = seq * S
            for h in range(NH):
                hb = (seq * NH + h) % 2
                if h % 8 == 0:
                    kvb = kvc % 2
                    kvc += 1
                    kvh = h // 8
                    self.DMA("sync", kT[kvb], FT.ap()[1024 + kvh * 64:1024 + (kvh + 1) * 64, tok0:tok0 + S], [], ["kT%d" % kvb])
                    self.DMA("sync", vx[kvb][:, :, 0:64],
                             TM.ap()[tok0:tok0 + S, kvh * 64:(kvh + 1) * 64].rearrange("(t p) c -> p t c", p=128),
                             [], ["vx%d" % kvb])
                self.DMA("sync", qT[hb], FT.ap()[h * 64:(h + 1) * 64, tok0:tok0 + S], [], ["qT%d" % hb])
                self.DMA("sync", EB[hb], self.eb_ap(0, h), [], ["EB%d" % hb])

                def fin(m, acc, kacc, h=h):
                    d = den[self.acc_cnt % 2]
                    kd = "den%d" % (self.acc_cnt % 2)
                    self.TS("vector", d[:, 0:4], acc[:, :, 64], esink[:, h:h + 1], None, ALU.add, None, [kacc, "esink"], [kd])
                    self.RECIP(d[:, 4:8], d[:, 0:4], [kd], [kd])
                    self.TT("vector", O_sb[:, 4 * m:4 * m + 4, h * 64:(h + 1) * 64], acc[:, :, 0:64],
                            d[:, 4:8].unsqueeze(2).broadcast_to([128, 4, 64]), ALU.mult, [kacc, kd], ["O"])

                if getattr(self, "dbg", "") == "LOADS":
                    continue
                self.attend(qTf[hb], "qT%d" % hb, kTf[kvb], "kT%d" % kvb, vx[kvb], "vx%d" % kvb,
                            EB[hb], "EB%d" % hb, 128, fin)
            if getattr(self, "dbg", "") in ("LOADS", "ATT"):
                continue
            self.outproj(seq, O_sb, "O", wo, g_out, src, dst, bufs)


    def mixer_C(self, layer, j, src, dst, FT, TM):
        A = self.A
        w_in = self.din("c_w_in", [1, D, 3072]).ap()[j]
        w_out = self.din("c_w_out", [1, D, D]).ap()[j]
        mobac = self.din("mobac", [128, 3, 128])
        fm = [(g * 128, g * 128) for g in range(16)]
        tmc = [(2048, 512, 0), (2560, 512, 512)]
        self.proj(src, layer, w_in, 3072, fm, tmc, FT, TM)
        self.em.barrier()
        A.off = self.base_off
        g_out, wo, O_sb, bufs = self.attn_common(layer, w_out)
        cst = A.f32(3 * 128).rearrange("p (a c) -> p a c", a=3)
        self.DMA("sync", cst, mobac.ap(), [], ["cst"])
        PASTc = cst[:, 0, :].rearrange("p (t n) -> p t n", n=8)
        NEGBc = cst[:, 1, :].rearrange("p (t n) -> p t n", n=8)
        OWNc = cst[:, 2, :].rearrange("p (t n) -> p t n", n=8)
        FULL = 1
        qTf = [A.bf16(S) for _ in range(2)]
        kTf = [A.bf16(S) for _ in range(2)]
        for b in range(2):
            self.MEMSET("vector", qTf[b], 0.0, ["qT%d" % b])
            self.MEMSET("vector", kTf[b], 0.0, ["kT%d" % b])
        qT = [t[0:64, :] for t in qTf]
        kT = [t[0:64, :] for t in kTf]
        VW = 128 if FULL else 65
        vx = [A.bf16(16 * VW).rearrange("p (t c) -> p t c", t=16) for _ in range(2)]
        EB = [A.bf16(EBW) for _ in range(2)]
        den = [A.f32(8) for _ in range(2)]
        km32 = A.f32(8)[0:64, :]
        kmhi = A.bf16(8)[0:64, :]
        kmlo = A.bf16(8)[0:64, :]
        kmr = A.f32(8)[0:64, :]
        gm = A.f32(128).rearrange("p (t n) -> p t n", n=8)
        top8 = A.f32(128).rearrange("p (t n) -> p t n", n=8)
        sel = A.f32(128).rearrange("p (t n) -> p t n", n=8)
        mneg = A.bf16(128).rearrange("p (t n) -> p t n", n=8)
        MnegTf = [A.bf16(S) for _ in range(2)]
        MnegT = [t[0:8, :] for t in MnegTf]
        for b in range(2):
            self.MEMSET("vector", MnegTf[b], 0.0, ["MnegT%d" % b])
            if FULL:
                self.MEMSET("vector", vx[b], 0.0, ["vx%d" % b])
            self.MEMSET("vector", vx[b][:, :, 0:65], 1.0, ["vx%d" % b])
        ident = self.ident
        pg = self.pb(5)[:, 0:128].rearrange("p (t n) -> p t n", n=8)
        for seq in range(self.nseq):
            tok0 = seq * S
            for h in range(NH):
                hb = (seq * NH + h) % 2
                self.DMA("sync", kT[hb], FT.ap()[1024 + h * 64:1024 + (h + 1) * 64, tok0:tok0 + S], [], ["kT%d" % hb])
                self.DMA("sync", vx[hb][:, :, 0:64],
                         TM.ap()[tok0:tok0 + S, h * 64:(h + 1) * 64].rearrange("(t p) c -> p t c", p=128),
                         [], ["vx%d" % hb])
                self.DMA("sync", qT[hb], FT.ap()[h * 64:(h + 1) * 64, tok0:tok0 + S], [], ["qT%d" % hb])
                self.DMA("sync", EB[hb], self.eb_ap(1, h), [], ["EB%d" % hb])
                self.em.op("vector", lambda e, o=km32, i=kT[hb].rearrange("p (n k) -> p n k", n=8):
                           e.tensor_reduce(out=o, in_=i, axis=AX.X, op=ALU.add), ["kT%d" % hb], ["km"])
                self.TS("vector", km32, km32, 1.0 / 256, None, ALU.mult, None, ["km"], ["km"])
                self.CP("vector", kmhi, km32, ["km"], ["kmhi"])
                self.TT("vector", kmr, km32, kmhi, ALU.subtract, ["km", "kmhi"], ["kmr"])
                self.CP("vector", kmlo, kmr, ["kmr"], ["kmlo"])
                for qt in range(16):
                    self.MM(pg[:, qt, :], qT[hb][:, qt * 128:(qt + 1) * 128], kmhi, True, False,
                            ["qT%d" % hb, "kmhi"], [("ps", 5)])
                    self.MM(pg[:, qt, :], qT[hb][:, qt * 128:(qt + 1) * 128], kmlo, False, True,
                            ["qT%d" % hb, "kmlo"], [("ps", 5)])
                self.TT("vector", gm, pg, PASTc, ALU.mult, [("ps", 5), "cst"], ["gm"])
                self.TT("vector", gm, gm, NEGBc, ALU.add, ["gm", "cst"], ["gm"])
                for qt in range(16):
                    self.em.op("vector", lambda e, o=top8[:, qt, :], i=gm[:, qt, :]: e.max(out=o, in_=i), ["gm"], ["top8"])
                self.TT("vector", sel, gm, top8[:, :, 2:3].broadcast_to([128, 16, 8]), ALU.is_ge, ["gm", "top8"], ["sel"])
                self.TT("vector", sel, sel, OWNc, ALU.max, ["sel", "cst"], ["sel"])
                self.TS("vector", mneg, sel, 30000.0, -30000.0, ALU.mult, ALU.add, ["sel"], ["mneg"])
                pmt = self.pb(6).bitcast(BF16)
                for half in range(2):
                    for i in range(8):
                        qt = half * 8 + i
                        self.TR(pmt[0:8, i * 128:(i + 1) * 128], mneg[:, qt, :], ["mneg"], [("ps", 6)])
                    self.CP("scalar", MnegT[hb][:, half * 1024:(half + 1) * 1024], pmt[0:8, :], [("ps", 6)], ["MnegT%d" % hb])

                def fin(m, acc, kacc, h=h):
                    d = den[self.acc_cnt % 2]
                    kd = "den%d" % (self.acc_cnt % 2)
                    self.RECIP(d[:, 4:8], acc[:, :, 64], [kacc], [kd])
                    self.TT("vector", O_sb[:, 4 * m:4 * m + 4, h * 64:(h + 1) * 64], acc[:, :, 0:64],
                            d[:, 4:8].unsqueeze(2).broadcast_to([128, 4, 64]), ALU.mult, [kacc, kd], ["O"])

                def rowfn(kt):
                    n = kt // 2
                    if FULL:
                        return ident[:, n:n + 1].broadcast_to([128, 128])
                    return ident[0:8, n:n + 1].broadcast_to([8, 128])

                if FULL:
                    self.attend(qTf[hb], "qT%d" % hb, kTf[hb], "kT%d" % hb, vx[hb], "vx%d" % hb,
                                EB[hb], "EB%d" % hb, None, fin, mask=(MnegTf[hb], "MnegT%d" % hb, rowfn))
                else:
                    self.attend(qT[hb], "qT%d" % hb, kT[hb], "kT%d" % hb, vx[hb], "vx%d" % hb,
                                EB[hb], "EB%d" % hb, None, fin, mask=(MnegT[hb], "MnegT%d" % hb, rowfn))
            self.outproj(seq, O_sb, "O", wo, g_out, src, dst, bufs)


    def mixer_B(self, layer, j, src, dst, FT, TM):
        A = self.A
        em = self.em
        ident = self.ident
        w_in = self.din("b_w_in", [1, D, 2608]).ap()[j]
        w_out = self.din("b_w_out", [1, D, D]).ap()[j]
        cpos_d = self.din("b_cmp_pos", [1, 2, 32, 64]).ap()[j]
        w1_d = self.din("b_cmp_w1", [1, 2, 2048, 256]).ap()[j]
        w2_d = self.din("b_cmp_w2", [1, 2, 256, 64]).ap()[j]
        cmask_d = self.din("cmask", [127, S])
        ovl_d = self.din("ovl", [127, 32])
        selc_d = self.din("selc", [128, 2, 512])
        fm = [(g * 128, g * 128) for g in range(8)]
        fm += [(1024, 1024), (1152, 1152), (1280, 1280), (1408, 1408), (1536, 1536), (1664, 1664),
               (2048, 1792), (2176, 1920)]
        tmc = [(1792, 256, 0), (2304, 304, 256)]
        self.proj(src, layer, w_in, 2608, fm, tmc, FT, TM)
        self.em.barrier()
        A.off = self.base_off
        kcbTf = [A.bf16(4 * 128).rearrange("p (g n) -> p g n", g=4) for _ in range(self.nseq)]
        Rvf = [A.bf16(4 * 128).rearrange("p (g c) -> p g c", g=4) for _ in range(self.nseq)]
        kcbT = [t[0:64, :, 0:127] for t in kcbTf]
        Rv = [t[0:127, :, 0:97] for t in Rvf]
        keep_off = A.off
        ovl_sb = A.f32(32)[0:127, :]
        self.DMA("sync", ovl_sb, ovl_d.ap(), [], ["ovl"])
        for sq in range(self.nseq):
            self.MEMSET("vector", kcbTf[sq], 0.0, ["kcbT%d" % sq])
            self.MEMSET("vector", Rvf[sq], 0.0, ["Rv%d" % sq])
            self.MEMSET("vector", Rv[sq], 1.0, ["Rv%d" % sq])
            for g in range(4):
                self.CP("vector", Rv[sq][:, g, 65:97], ovl_sb, ["ovl", "Rv%d" % sq], ["Rv%d" % sq])
        w1 = A.bf16(32 * 256)[0:64, :].rearrange("p (l j) -> p l j", l=32)
        w2 = A.bf16(2 * 64).rearrange("p (h d) -> p h d", h=2)
        posn = A.f32(64)[0:32, :]
        posb = A.bf16(64)[0:32, :]
        posT = A.bf16(32)[0:64, :]
        cb = A.f32(2)
        xT = A.bf16(4 * S)[0:64, :].rearrange("p (g t) -> p g t", g=4)
        xh = A.f32(508)
        x2 = A.f32(508)
        sgm = A.f32(508)
        hid = [A.bf16(508) for _ in range(2)]
        pstb = self.pb(7).bitcast(BF16)
        for kv in range(2):
            self.DMA("gpsimd", w1, w1_d[kv].rearrange("(l d) j -> d l j", d=64), [], ["w1"])
            self.DMA("gpsimd", w2, w2_d[kv].rearrange("(h p) d -> p h d", p=128), [], ["w2"])
            self.DMA("sync", posn, cpos_d[kv], [], ["posn"])
            self.CP("vector", posb, posn, ["posn"], ["posb"])
            self.TR(pstb[0:64, 0:32], posb, ["posb"], [("ps", 7)])
            self.CP("scalar", posT, pstb[0:64, 0:32], [("ps", 7)], ["posT"])
            for jh in range(2):
                for l in range(32):
                    self.MM(self.pb(6)[:, jh:jh + 1], w1[:, l, jh * 128:(jh + 1) * 128], posT[:, l:l + 1],
                            l == 0, l == 31, ["w1", "posT"], [("ps", 6)])
            self.CP("vector", cb, self.pb(6)[:, 0:2], [("ps", 6)], ["cb"])
            for sq in range(self.nseq):
                tok0 = sq * S
                r0 = 1024 if kv == 0 else 1280
                self.DMA("sync", xT, FT.ap()[r0:r0 + 256, tok0:tok0 + S].rearrange("(g d) t -> d g t", d=64), [], ["xT"])
                for jh in range(2):
                    bank = jh
                    ph = self.pb(bank)[:, 0:508].rearrange("p (g n) -> p g n", g=4)
                    for g in range(4):
                        for l in range(32):
                            self.MM(ph[:, g, :], w1[:, l, jh * 128:(jh + 1) * 128], xT[:, g, l:l + 16 * 126 + 1:16],
                                    l == 0, l == 31, ["w1", "xT"], [("ps", bank)])
                    phf = self.pb(bank)[:, 0:508]
                    self.TS("vector", xh, phf, cb[:, jh:jh + 1], None, ALU.add, None, [("ps", bank), "cb"], ["xh"])
                    self.TT("vector", x2, xh, xh, ALU.mult, ["xh"], ["x2"])
                    self.TS("vector", x2, x2, 0.044715, 1.0, ALU.mult, ALU.add, ["x2"], ["x2"])
                    self.TT("vector", x2, x2, xh, ALU.mult, ["x2", "xh"], ["x2"])
                    self.ACT(sgm, x2, AF.Sigmoid, ["x2"], ["sgm"], scale=1.5957691216057308)
                    self.TT("vector", hid[jh], sgm, xh, ALU.mult, ["sgm", "xh"], ["hid%d" % jh])
                if kv == 0:
                    po = self.pb(2)[0:64, 0:508]
                    for jh in range(2):
                        self.MM(po, w2[:, jh, :], hid[jh], jh == 0, jh == 1, ["w2", "hid%d" % jh], [("ps", 2)])
                    self.CP("vector", kcbT[sq], po.rearrange("p (g n) -> p g n", g=4), [("ps", 2)], ["kcbT%d" % sq])
                else:
                    po = self.pb(2)[0:127, 0:256].rearrange("p (g d) -> p g d", g=4)
                    for g in range(4):
                        for jh in range(2):
                            self.MM(po[:, g, :], hid[jh][:, g * 127:(g + 1) * 127], w2[:, jh, :], jh == 0, jh == 1,
                                    ["w2", "hid%d" % jh], [("ps", 2)])
                    self.CP("vector", Rv[sq][:, :, 0:64], po, [("ps", 2)], ["Rv%d" % sq])
        self.em.barrier()
        A.off = keep_off
        g_out, wo, O_sb, bufs = self.attn_common(layer, w_out)
        CMf = A.bf16(S)
        CM = CMf[0:127, :]
        self.MEMSET("vector", CMf, 0.0, ["CM"])
        self.DMA("gpsimd", CM, cmask_d.ap(), [], ["CM"])
        selc = A.f32(1024).rearrange("p (a t j) -> p a t j", a=2, t=16)
        self.DMA("sync", selc.rearrange("p a t j -> p a (t j)"), selc_d.ap(), [], ["selc"])
        qTgf = [A.bf16(S) for _ in range(4)]
        ksTf = A.bf16(S)
        kwTf = A.bf16(S)
        qTg = [t[0:64, :] for t in qTgf]
        ksT = ksTf[0:64, :]
        kwT = kwTf[0:64, :]
        vs = A.bf16(16 * 128).rearrange("p (t c) -> p t c", t=16)
        vw = A.bf16(16 * 128).rearrange("p (t c) -> p t c", t=16)
        for r in range(4):
            self.MEMSET("vector", qTgf[r], 0.0, ["qT%d" % r])
        self.MEMSET("vector", ksTf, 0.0, ["ksT"])
        self.MEMSET("vector", kwTf, 0.0, ["kwT"])
        self.MEMSET("vector", vs, 0.0, ["vs"])
        self.MEMSET("vector", vw, 0.0, ["vw"])
        self.MEMSET("vector", vs[:, :, 0:65], 1.0, ["vs"])
        self.MEMSET("vector", vw[:, :, 0:65], 1.0, ["vw"])
        EB = [A.bf16(EBW) for _ in range(2)]
        graw = A.bf16(16 * 48).rearrange("p (t c) -> p t c", t=16)
        gsig = A.f32(16 * 48).rearrange("p (t c) -> p t c", t=16)
        imp = A.f32(16 * 32).rearrange("p (t j) -> p t j", t=16)
        impw = A.f32(16 * 32).rearrange("p (t j) -> p t j", t=16)
        impx = A.f32(16 * 32).rearrange("p (t j) -> p t j", t=16)
        m1 = A.f32(128).rearrange("p (t n) -> p t n", n=8)
        m2 = A.f32(128).rearrange("p (t n) -> p t n", n=8)
        mneg = A.bf16(16 * 32).rearrange("p (t j) -> p t j", t=16)
        MnegTf = A.bf16(S)
        MnegT = MnegTf[0:32, :]
        self.MEMSET("vector", MnegTf, 0.0, ["MnegT"])
        dd = [A.f32(16) for _ in range(2)]
        tb = [A.f32(256).rearrange("p (i d) -> p i d", i=4) for _ in range(2)]
        tcnt = [0]
        ebc = [0]

        Esel = A.bf16(16 * 128).rearrange("p (t k) -> p t k", t=16)
        for kt in range(16):
            self.CP("vector", Esel[:, kt, :].rearrange("p (a b) -> p a b", a=2),
                    ident[:, 2 * kt:2 * kt + 2].unsqueeze(2).broadcast_to([128, 2, 64]), ["ident"], ["Esel"])

        def rowfn(kt):
            return Esel[:, kt, :]

        for seq in range(self.nseq):
            tok0 = seq * S
            self.DMA("sync", graw, TM.ap()[tok0:tok0 + S, 512:560].rearrange("(t p) c -> p t c", p=128), [], ["graw"])
            self.ACT(gsig, graw, AF.Sigmoid, ["graw"], ["gsig"])
            for g in range(4):
                for r in range(4):
                    h = 4 * g + r
                    self.DMA("sync", qTg[r], FT.ap()[h * 64:(h + 1) * 64, tok0:tok0 + S], [], ["qT%d" % r])
                self.DMA("sync", ksT, FT.ap()[1536 + g * 64:1536 + (g + 1) * 64, tok0:tok0 + S], [], ["ksT"])
                self.DMA("sync", kwT, FT.ap()[1792 + g * 64:1792 + (g + 1) * 64, tok0:tok0 + S], [], ["kwT"])
                self.DMA("sync", vs[:, :, 0:64],
                         TM.ap()[tok0:tok0 + S, g * 64:(g + 1) * 64].rearrange("(t p) c -> p t c", p=128), [], ["vs"])
                self.DMA("sync", vw[:, :, 0:64],
                         TM.ap()[tok0:tok0 + S, 256 + g * 64:256 + (g + 1) * 64].rearrange("(t p) c -> p t c", p=128),
                         [], ["vw"])
                citems = [dict(r=r, m=m) for r in range(4) for m in range(4)]

                def cstage1(it):
                    r, m = it["r"], it["m"]
                    q0 = 512 * m
                    sb = self.st_cnt % 3
                    self.st_cnt += 1
                    it["sb"] = sb
                    ps = self.pb(sb)
                    kps = ("ps", sb)
                    self.MM(ps, kcbTf[seq][:, g, :], qTgf[r][:, q0:q0 + 512], True, True,
                            ["kcbT%d" % seq, "qT%d" % r], [kps])
                    self.ACT(self.E[sb], ps, AF.Exp, [kps], ["E%d" % sb], scale=0.125)
                    self.TT("vector", self.PT[sb], self.E[sb], CMf[:, q0:q0 + 512], ALU.mult,
                            ["E%d" % sb, "CM"], ["PT%d" % sb])

                def cstage2(it):
                    r, m, sb = it["r"], it["m"], it["sb"]
                    h = 4 * g + r
                    cbk = 5 + tcnt[0] % 2
                    kcb = ("ps", cbk)
                    accC = self.pb(cbk).rearrange("p (i c) -> p i c", i=4)
                    for i in range(4):
                        self.MM(accC[:, i, :], self.PT[sb][:, i * 128:(i + 1) * 128], Rvf[seq][:, g, :],
                                True, True, ["PT%d" % sb, "Rv%d" % seq], [kcb])
                    d = dd[tcnt[0] % 2]
                    kd = "dd%d" % (tcnt[0] % 2)
                    tcnt[0] += 1
                    self.TS("vector", d[:, 0:4], accC[:, :, 64], 1e-30, None, ALU.max, None, [kcb], [kd])
                    self.RECIP(d[:, 4:8], d[:, 0:4], [kd], [kd])
                    self.TT("vector", d[:, 8:12], d[:, 4:8], gsig[:, 4 * m:4 * m + 4, 3 * h], ALU.mult, [kd, "gsig"], [kd])
                    self.TT("vector", O_sb[:, 4 * m:4 * m + 4, h * 64:(h + 1) * 64], accC[:, :, 0:64],
                            d[:, 8:12].unsqueeze(2).broadcast_to([128, 4, 64]), ALU.mult, [kcb, kd], ["O"])
                    rb = d[:, 4:8].unsqueeze(2).broadcast_to([128, 4, 32])
                    if r == 0:
                        self.TT("vector", imp[:, 4 * m:4 * m + 4, :], accC[:, :, 65:97], rb, ALU.mult,
                                [kcb, kd], ["imp"])
                    else:
                        self.TT("vector", impx[:, 4 * m:4 * m + 4, :], accC[:, :, 65:97], rb, ALU.mult,
                                [kcb, kd], ["impx"])
                        self.TT("vector", imp[:, 4 * m:4 * m + 4, :], imp[:, 4 * m:4 * m + 4, :],
                                impx[:, 4 * m:4 * m + 4, :], ALU.add, ["imp", "impx"], ["imp"])

                for ci in range(len(citems) + 2):
                    if ci < len(citems):
                        cstage1(citems[ci])
                    if ci - 2 >= 0:
                        cstage2(citems[ci - 2])
                self.TT("vector", impw, imp, selc[:, 0], ALU.mult, ["imp", "selc"], ["impw"])
                self.TT("vector", impw, impw, selc[:, 1], ALU.add, ["impw", "selc"], ["impw"])
                for qt in range(16):
                    em.op("vector", lambda e, o=m1[:, qt, :], i=impw[:, qt, :]: e.max(out=o, in_=i), ["impw"], ["m1"])
                    em.op("vector", lambda e, o=impx[:, qt, :], t=m1[:, qt, :], i=impw[:, qt, :]:
                          e.match_replace(out=o, in_to_replace=t, in_values=i, imm_value=-3e9), ["impw", "m1"], ["impx"])
                    em.op("vector", lambda e, o=m2[:, qt, :], i=impx[:, qt, :]: e.max(out=o, in_=i), ["impx"], ["m2"])
                self.TT("vector", impx, impw, m2[:, :, 7:8].broadcast_to([128, 16, 32]), ALU.is_ge, ["impw", "m2"], ["impx"])
                self.TS("vector", mneg, impx, 30000.0, -30000.0, ALU.mult, ALU.add, ["impx"], ["mneg"])
                pmt = self.pb(7).bitcast(BF16)
                for half in range(2):
                    for i in range(8):
                        qt = half * 8 + i
                        self.TR(pmt[0:32, i * 128:(i + 1) * 128], mneg[:, qt, :], ["mneg"], [("ps", 7)])
                    self.CP("scalar", MnegT[:, half * 1024:(half + 1) * 1024], pmt[0:32, :], [("ps", 7)], ["MnegT"])
                for br in (1, 2):
                    for r in range(4):
                        h = 4 * g + r
                        eb = ebc[0] % 2
                        ebc[0] += 1
                        self.DMA("sync", EB[eb], self.eb_ap(br, h), [], ["EB%d" % eb])

                        def fin(m, acc, kacc, h=h, br=br):
                            d = dd[tcnt[0] % 2]
                            kd = "dd%d" % (tcnt[0] % 2)
                            t_ = tb[tcnt[0] % 2]
                            kt_ = "tb%d" % (tcnt[0] % 2)
                            tcnt[0] += 1
                            self.TS("vector", d[:, 0:4], acc[:, :, 64], 1e-30, None, ALU.max, None, [kacc], [kd])
                            self.RECIP(d[:, 4:8], d[:, 0:4], [kd], [kd])
                            self.TT("vector", d[:, 8:12], d[:, 4:8], gsig[:, 4 * m:4 * m + 4, 3 * h + br], ALU.mult,
                                    [kd, "gsig"], [kd])
                            self.TT("vector", t_, acc[:, :, 0:64], d[:, 8:12].unsqueeze(2).broadcast_to([128, 4, 64]),
                                    ALU.mult, [kacc, kd], [kt_])
                            osl = O_sb[:, 4 * m:4 * m + 4, h * 64:(h + 1) * 64]
                            self.TT("vector", osl, osl, t_, ALU.add, ["O", kt_], ["O"])

                        if br == 1:
                            self.attend(qTgf[r], "qT%d" % r, ksTf, "ksT", vs, "vs", EB[eb], "EB%d" % eb, None, fin,
                                        mask=(MnegTf, "MnegT", rowfn))
                        else:
                            self.attend(qTgf[r], "qT%d" % r, kwTf, "kwT", vw, "vw", EB[eb], "EB%d" % eb, 512, fin)
            self.outproj(seq, O_sb, "O", wo, g_out, src, dst, bufs)

def bucket_np(dist):
    dist = np.maximum(dist, 0)
    dd = np.maximum(dist, 1).astype(np.float32)
    lb = 16 + (np.log(dd / np.float32(16)) / np.float32(math.log(64)) * np.float32(16)).astype(np.int32)
    return np.where(dist < 16, dist, np.minimum(lb, 31))


def host_consts():
    U = REL_U
    dist = np.arange(U) - 127
    bk = bucket_np(dist)
    oh = np.zeros((3, 33, U), np.float32)
    for ty, win in enumerate((128, None, 512)):
        valid = dist >= 0
        if win is not None:
            valid &= dist < win
        for u in range(U):
            if valid[u]:
                oh[ty, bk[u], u] = 1.0
            else:
                oh[ty, 32, u] = 1.0
    mc = np.zeros((128, 3, 16, 8), np.float32)
    for qt in range(16):
        for n in range(8):
            past = 1.0 if n < qt // 2 else 0.0
            mc[:, 0, qt, n] = past
            mc[:, 1, qt, n] = -1e9 * (1.0 - past)
            mc[:, 2, qt, n] = 1.0 if n == qt // 2 else 0.0
    n_cmp = 127
    q = np.arange(S)
    cmask = ((np.arange(n_cmp)[:, None] * 16 + 31) <= q[None, :]).astype(np.float32)
    c_start = np.arange(n_cmp)[:, None] * 16
    s_start = np.arange(32)[None, :] * 64
    ovl = ((c_start < s_start + 64) & (c_start + 32 > s_start)).astype(np.float32)
    selc = np.zeros((128, 2, 16, 32), np.float32)
    for qt in range(16):
        for p in range(128):
            cur = (qt * 128 + p) // 64
            for jb in range(32):
                if jb > cur:
                    selc[p, 0, qt, jb] = 0.0
                    selc[p, 1, qt, jb] = -1e9 - 1024.0 * jb
                elif jb == 0 or jb == cur or jb == cur - 1:
                    selc[p, 0, qt, jb] = 0.0
                    selc[p, 1, qt, jb] = 1e9 + 1024.0 * jb
                else:
                    selc[p, 0, qt, jb] = 1.0
    return {"oh": oh, "mobac": mc.reshape(128, 3, 128), "cmask": cmask, "ovl": ovl,
            "selc": selc.reshape(128, 2, 512)}


FULL_PLAN = [("rel",)]
for _l in range(4):
    FULL_PLAN += [("ffn", _l, 0), ("mix", _l), ("ffn", _l, 1)]


def kernel(**inputs):
    n = 8
    nseq = 2
    b = Builder(nseq, FULL_PLAN)
    nc = b.build()
    x = np.ascontiguousarray(inputs["x"], dtype=np.float32)
    full = dict(inputs)
    full.update(host_consts())
    shared = {name: np.ascontiguousarray(full[name], dtype=np.float32) for name in b.ext_inputs if name != "x"}
    in_maps = []
    for c in range(n):
        m = dict(shared)
        m["x"] = np.ascontiguousarray(x[c * nseq:(c + 1) * nseq].reshape(nseq * S, D))
        in_maps.append(m)
    res = run_bass_kernel_spmd(nc, in_maps, core_ids=list(range(n)))
    out = np.concatenate([r["y"].reshape(nseq, S, D) for r in res.results], axis=0)
    return out.astype(np.float32)
```
